# Optimizing a Trainium2 kernel written in Bass

```python
import math
import jax
import jax.numpy as jnp
from jax import lax
import numpy as np

D_MODEL = 1024
BATCH = 2
SEQ = 8192
DEPTH = 1

SSM_EXPAND = 2
D_INNER = SSM_EXPAND * D_MODEL
SSM_HEAD_DIM = 64
SSM_HEADS = D_INNER // SSM_HEAD_DIM
SSM_GROUPS = 2
D_STATE = 128
CONV_K = 4
CONV_DIM = D_INNER + 2 * SSM_GROUPS * D_STATE
SSD_CHUNK = 128
ATTN_HEAD_DIM = 64
DILATED_CONFIGS = ((128, 1), (512, 4), (2048, 16))
HEADS_PER_DIL_GROUP = 8
ATTN_HEADS = HEADS_PER_DIL_GROUP * 3
ATTN_WIDTH = ATTN_HEADS * ATTN_HEAD_DIM
ATTN_OUT_WIDTH = HEADS_PER_DIL_GROUP * ATTN_HEAD_DIM
NUM_BUCKETS = 32
MAX_DISTANCE = 2048
N_EXPERT_GROUPS = 8
EXPERTS_PER_GROUP = 8
N_EXPERTS = N_EXPERT_GROUPS * EXPERTS_PER_GROUP
TOP_K_FINE = 2
D_EXPERT = 512
MOE_BLOCK = 128
NORM_EPS = 1e-6

IN_SPLITS = (D_INNER, CONV_DIM, SSM_HEADS, ATTN_WIDTH, ATTN_WIDTH, ATTN_WIDTH, D_MODEL, D_MODEL)
IN_TOTAL = D_INNER + CONV_DIM + SSM_HEADS + 3 * ATTN_WIDTH + 2 * D_MODEL

kernel_name = 'hybrid_ssd_dilated_attn_hmoe_layer'


def rmsnorm(x, w):
    xf = x.astype(jnp.float32)
    y = xf * lax.rsqrt(jnp.mean(xf * xf, axis=-1, keepdims=True) + NORM_EPS)
    return (y * w.astype(jnp.float32)).astype(x.dtype)


def segsum(a):
    t = a.shape[-1]
    a_rep = jnp.broadcast_to(a[..., :, None], a.shape + (t,))
    a_rep = jnp.where(np.tril(np.ones((t, t), dtype=bool), -1), a_rep, 0.0)
    cs = jnp.cumsum(a_rep, axis=-2)
    return jnp.where(np.tril(np.ones((t, t), dtype=bool)), cs, -jnp.inf)


def ssd_chunked(xdt, a_dt, bm, cm):
    b, s, h, p = xdt.shape
    g, n = bm.shape[2], bm.shape[3]
    hg = h // g
    c, l = s // SSD_CHUNK, SSD_CHUNK
    xc = xdt.reshape(b, c, l, g, hg, p)
    ac = a_dt.reshape(b, c, l, g, hg).transpose(0, 3, 4, 1, 2)
    bc = bm.reshape(b, c, l, g, n)
    cc = cm.reshape(b, c, l, g, n)
    a_cs = jnp.cumsum(ac, axis=-1)
    decay_in = jnp.exp(segsum(ac))
    cb = jnp.einsum('bclgn,bcsgn->bgcls', cc, bc)
    y_diag = jnp.einsum('bghcls,bcsghp->bclghp', cb[:, :, None] * decay_in, xc)
    decay_to_end = jnp.exp(a_cs[..., -1:] - a_cs).transpose(0, 3, 4, 1, 2)
    states = jnp.einsum('bclgn,bclghp->bcghpn', bc, xc * decay_to_end[..., None])
    states = jnp.concatenate([jnp.zeros_like(states[:, :1]), states], axis=1)
    chunk_tot = jnp.pad(a_cs[..., -1], ((0, 0), (0, 0), (0, 0), (1, 0)))
    chunk_decay = jnp.exp(segsum(chunk_tot))
    states = jnp.einsum('bghzc,bcghpn->bzghpn', chunk_decay, states)[:, :-1]
    decay_from_start = jnp.exp(a_cs).transpose(0, 3, 4, 1, 2)
    y_off = jnp.einsum('bclgn,bcghpn->bclghp', cc, states) * decay_from_start[..., None]
    return (y_diag + y_off).reshape(b, s, h, p)


def ssd_mixer(z, xbc, dt_raw, conv_w, conv_b, dt_bias, a_log, d_skip, ssm_norm_w):
    b, s, _ = xbc.shape
    f32 = jnp.float32
    xbc = lax.conv_general_dilated(xbc, conv_w[:, None, :].astype(xbc.dtype), window_strides=(1,),
                                   padding=[(CONV_K - 1, 0)], dimension_numbers=('NWC', 'WIO', 'NWC'),
                                   feature_group_count=CONV_DIM)
    xbc = jax.nn.silu(xbc + conv_b.astype(xbc.dtype)).astype(f32)
    xs, bm, cm = jnp.split(xbc, [D_INNER, D_INNER + SSM_GROUPS * D_STATE], axis=-1)
    xs = xs.reshape(b, s, SSM_HEADS, SSM_HEAD_DIM)
    bm = bm.reshape(b, s, SSM_GROUPS, D_STATE)
    cm = cm.reshape(b, s, SSM_GROUPS, D_STATE)
    dt = jax.nn.softplus(dt_raw.astype(f32) + dt_bias.astype(f32))
    a = -jnp.exp(a_log.astype(f32))
    y = ssd_chunked(xs * dt[..., None], dt * a, bm, cm) + d_skip.astype(f32)[:, None] * xs
    y = y.reshape(b, s, D_INNER) * jax.nn.silu(z.astype(f32))
    y = y.reshape(b, s, SSM_GROUPS, D_INNER // SSM_GROUPS)
    y = y * lax.rsqrt(jnp.mean(y * y, axis=-1, keepdims=True) + NORM_EPS)
    y = y.reshape(b, s, D_INNER) * ssm_norm_w.astype(f32)
    return y.astype(z.dtype)


def t5_causal_bucket(dist):
    max_exact = NUM_BUCKETS // 2
    large = max_exact + (np.log(np.maximum(dist, max_exact) / max_exact)
                         / math.log(MAX_DISTANCE / max_exact) * (NUM_BUCKETS - max_exact)).astype(np.int32)
    return np.where(dist < max_exact, dist, np.minimum(large, NUM_BUCKETS - 1)).astype(np.int32)


def dilated_band_attention(q, k, v, bias_tab, window, dilation):
    b, s, h, dh = q.shape
    f32 = jnp.float32
    blk = window // dilation
    span = blk * dilation
    s_pad = -(-s // span) * span
    nb = s_pad // span

    def to_residue_blocks(t):
        t = jnp.pad(t, ((0, 0), (0, s_pad - s), (0, 0), (0, 0)))
        t = t.reshape(b, s_pad // dilation, dilation, h, dh).transpose(0, 2, 1, 3, 4)
        return t.reshape(b, dilation, nb, blk, h, dh).astype(f32)

    def band(t):
        prev = jnp.concatenate([jnp.zeros_like(t[:, :, :1]), t[:, :, :-1]], axis=2)
        return jnp.concatenate([prev, t], axis=3)

    qb = to_residue_blocks(q)
    kb = band(to_residue_blocks(k))
    vb = band(to_residue_blocks(v))
    off = np.arange(blk)[:, None] + blk - np.arange(2 * blk)[None, :]
    in_win = (off >= 0) & (off <= blk)
    first = (np.arange(nb)[:, None, None] == 0) & (np.arange(2 * blk)[None, None, :] < blk)
    valid = in_win[None] & ~first
    bucket = t5_causal_bucket(np.clip(off, 0, None) * dilation)
    bias = jnp.transpose(bias_tab[bucket], (2, 0, 1)).astype(f32)
    scores = jnp.einsum('brnqhd,brnkhd->brnhqk', qb, kb) * (dh ** -0.5) + bias
    scores = jnp.where(valid[:, None], scores, -jnp.inf)
    m = jnp.max(scores, axis=-1, keepdims=True)
    p = jnp.exp(scores - m)
    denom = jnp.sum(p, axis=-1)
    out = jnp.einsum('brnhqk,brnkhd->brnqhd', p, vb) / jnp.swapaxes(denom, -1, -2)[..., None]
    lse = jnp.swapaxes(m[..., 0] + jnp.log(denom), -1, -2)

    def from_residue_blocks(t):
        t = t.reshape((b, dilation, s_pad // dilation) + t.shape[4:])
        t = jnp.swapaxes(t, 1, 2)
        return t.reshape((b, s_pad) + t.shape[3:])[:, :s]

    return from_residue_blocks(out), from_residue_blocks(lse)


def dilated_attention_mixer(q, k, v, q_norm_w, k_norm_w, rel_bias):
    b, s, _ = q.shape
    shp = (b, s, ATTN_HEADS, ATTN_HEAD_DIM)
    q = rmsnorm(q.reshape(shp), q_norm_w)
    k = rmsnorm(k.reshape(shp), k_norm_w)
    v = v.reshape(shp)
    outs, lses = [], []
    for gi, (window, dilation) in enumerate(DILATED_CONFIGS):
        hs = slice(gi * HEADS_PER_DIL_GROUP, (gi + 1) * HEADS_PER_DIL_GROUP)
        o, lse = dilated_band_attention(q[:, :, hs], k[:, :, hs], v[:, :, hs], rel_bias[:, hs], window, dilation)
        outs.append(o)
        lses.append(lse)
    w = jax.nn.softmax(jnp.stack(lses), axis=0)
    out = jnp.sum(w[..., None] * jnp.stack(outs), axis=0)
    return out.reshape(b, s, ATTN_OUT_WIDTH).astype(v.dtype)


def hierarchical_moe(h, w_coarse, b_coarse, w_fine, b_fine, w_gate_exp, w_up_exp, w_down_exp):
    b, s, d = h.shape
    t = b * s
    hf = h.reshape(t, d)
    coarse_logits = (hf @ w_coarse + b_coarse).astype(jnp.float32)
    group = jnp.argmax(coarse_logits, axis=-1)
    group_p = jnp.take_along_axis(jax.nn.softmax(coarse_logits, axis=-1), group[:, None], axis=-1)
    fine_logits = (hf @ w_fine + b_fine).astype(jnp.float32).reshape(t, N_EXPERT_GROUPS, EXPERTS_PER_GROUP)
    fine_sel = jnp.take_along_axis(fine_logits, group[:, None, None], axis=1)[:, 0]
    top_p, top_i = lax.top_k(jax.nn.softmax(fine_sel, axis=-1), TOP_K_FINE)
    gates = group_p * top_p / jnp.sum(top_p, axis=-1, keepdims=True)
    eid = (group[:, None] * EXPERTS_PER_GROUP + top_i).reshape(-1)
    tok = jnp.repeat(jnp.arange(t), TOP_K_FINE)
    gw = gates.reshape(-1)
    order = jnp.argsort(eid)
    e_sorted, tok_sorted, gw_sorted = eid[order], tok[order], gw[order]
    counts = jnp.bincount(eid, length=N_EXPERTS)
    start = jnp.cumsum(counts) - counts
    padded = (counts + MOE_BLOCK - 1) // MOE_BLOCK * MOE_BLOCK
    pad_end = jnp.cumsum(padded)
    pad_start = pad_end - padded
    n_assign = t * TOP_K_FINE
    dest = pad_start[e_sorted] + jnp.arange(n_assign) - start[e_sorted]
    n_blocks = -(-(n_assign + N_EXPERTS * (MOE_BLOCK - 1)) // MOE_BLOCK)
    rows = jnp.zeros((n_blocks * MOE_BLOCK, d), h.dtype).at[dest].set(hf[tok_sorted])
    block_expert = jnp.minimum(jnp.searchsorted(pad_end, jnp.arange(n_blocks) * MOE_BLOCK, side='right'),
                               N_EXPERTS - 1)

    def run_block(args):
        xb, e = args
        hid = jax.nn.silu(xb @ w_gate_exp[e]) * (xb @ w_up_exp[e])
        return hid @ w_down_exp[e]

    y_rows = lax.map(run_block, (rows.reshape(n_blocks, MOE_BLOCK, d), block_expert)).reshape(-1, d)
    contrib = y_rows[dest] * gw_sorted[:, None].astype(h.dtype)
    return jnp.zeros((t, d), h.dtype).at[tok_sorted].add(contrib).reshape(b, s, d)


def hybrid_layer(x, norm_mix_w, w_in, conv_w, conv_b, dt_bias, a_log, d_skip, ssm_norm_w, w_ssm_proj,
                 q_norm_w, k_norm_w, rel_bias, w_attn_proj, w_out, norm_ffn_w, w_coarse, b_coarse,
                 w_fine, b_fine, w_gate_exp, w_up_exp, w_down_exp):
    h = rmsnorm(x, norm_mix_w)
    proj = h @ w_in
    split_points = np.cumsum(IN_SPLITS)[:-1].tolist()
    z, xbc, dt_raw, q, k, v, g_ssm, g_attn = jnp.split(proj, split_points, axis=-1)
    y_ssm = ssd_mixer(z, xbc, dt_raw, conv_w, conv_b, dt_bias, a_log, d_skip, ssm_norm_w) @ w_ssm_proj
    y_attn = dilated_attention_mixer(q, k, v, q_norm_w, k_norm_w, rel_bias) @ w_attn_proj
    merged = jax.nn.sigmoid(g_ssm) * y_ssm + jax.nn.sigmoid(g_attn) * y_attn
    x = x + merged @ w_out
    x = x + hierarchical_moe(rmsnorm(x, norm_ffn_w), w_coarse, b_coarse, w_fine, b_fine,
                             w_gate_exp, w_up_exp, w_down_exp)
    return x


def setup_inputs(seed: int = 0) -> dict:
    key = jax.random.key(seed)
    ks = jax.random.split(key, 23)
    f32 = jnp.float32

    def nrm(k, shape, scale):
        return jax.random.normal(k, shape, f32) * scale

    def gain(k, shape):
        return 1.0 + 0.02 * jax.random.normal(k, shape, f32)

    dt_init = jnp.exp(jax.random.uniform(ks[5], (DEPTH, SSM_HEADS), f32, math.log(1e-3), math.log(1e-1)))
    return {
        'x': nrm(ks[0], (BATCH, SEQ, D_MODEL), 1.0),
        'norm_mix_w': gain(ks[1], (DEPTH, D_MODEL)),
        'w_in': nrm(ks[2], (DEPTH, D_MODEL, IN_TOTAL), D_MODEL ** -0.5),
        'conv_w': nrm(ks[3], (DEPTH, CONV_K, CONV_DIM), CONV_K ** -0.5),
        'conv_b': nrm(ks[4], (DEPTH, CONV_DIM), 0.01),
        'dt_bias': dt_init + jnp.log(-jnp.expm1(-dt_init)),
        'a_log': jnp.log(jax.random.uniform(ks[6], (DEPTH, SSM_HEADS), f32, 1.0, 16.0)),
        'd_skip': gain(ks[7], (DEPTH, SSM_HEADS)),
        'ssm_norm_w': gain(ks[8], (DEPTH, D_INNER)),
        'w_ssm_proj': nrm(ks[9], (DEPTH, D_INNER, D_MODEL), D_INNER ** -0.5),
        'q_norm_w': gain(ks[10], (DEPTH, ATTN_HEAD_DIM)),
        'k_norm_w': gain(ks[11], (DEPTH, ATTN_HEAD_DIM)),
        'rel_bias': nrm(ks[12], (NUM_BUCKETS, ATTN_HEADS), 0.5),
        'w_attn_proj': nrm(ks[13], (DEPTH, ATTN_OUT_WIDTH, D_MODEL), ATTN_OUT_WIDTH ** -0.5),
        'w_out': nrm(ks[14], (DEPTH, D_MODEL, D_MODEL), D_MODEL ** -0.5),
        'norm_ffn_w': gain(ks[15], (DEPTH, D_MODEL)),
        'w_coarse': nrm(ks[16], (DEPTH, D_MODEL, N_EXPERT_GROUPS), D_MODEL ** -0.5),
        'b_coarse': nrm(ks[17], (DEPTH, N_EXPERT_GROUPS), 0.01),
        'w_fine': nrm(ks[18], (DEPTH, D_MODEL, N_EXPERTS), D_MODEL ** -0.5),
        'b_fine': nrm(ks[19], (DEPTH, N_EXPERTS), 0.01),
        'w_gate_exp': nrm(ks[20], (DEPTH, N_EXPERTS, D_MODEL, D_EXPERT), D_MODEL ** -0.5),
        'w_up_exp': nrm(ks[21], (DEPTH, N_EXPERTS, D_MODEL, D_EXPERT), D_MODEL ** -0.5),
        'w_down_exp': nrm(ks[22], (DEPTH, N_EXPERTS, D_EXPERT, D_MODEL), D_EXPERT ** -0.5),
    }


def reference(x, norm_mix_w, w_in, conv_w, conv_b, dt_bias, a_log, d_skip, ssm_norm_w, w_ssm_proj,
              q_norm_w, k_norm_w, rel_bias, w_attn_proj, w_out, norm_ffn_w, w_coarse, b_coarse,
              w_fine, b_fine, w_gate_exp, w_up_exp, w_down_exp):
    for layer in range(DEPTH):
        x = hybrid_layer(x, norm_mix_w[layer], w_in[layer], conv_w[layer], conv_b[layer], dt_bias[layer],
                         a_log[layer], d_skip[layer], ssm_norm_w[layer], w_ssm_proj[layer],
                         q_norm_w[layer], k_norm_w[layer], rel_bias, w_attn_proj[layer], w_out[layer],
                         norm_ffn_w[layer], w_coarse[layer], b_coarse[layer], w_fine[layer], b_fine[layer],
                         w_gate_exp[layer], w_up_exp[layer], w_down_exp[layer])
    return x
```

```python
import numpy as np
import concourse.bass as bass
import concourse.mybir as mybir
from concourse.bass_utils import run_bass_kernel_spmd

F32, BF16, I32 = mybir.dt.float32, mybir.dt.bfloat16, mybir.dt.int32
ALU = mybir.AluOpType
AF = mybir.ActivationFunctionType
AX = mybir.AxisListType

D = 1024
DI = 2048
NH = 32
TOK = 2048
PRE = 6144
NBLK = 16
EPS = 1e-6
C_Z, C_X, C_B, C_C, C_DT, C_Q, C_K, C_V, C_GS, C_GA = 0, 2048, 4096, 4352, 4608, 4640, 6176, 7712, 9248, 10272
NE = 64
CAP = 128
ENGS = ['pe', 'act', 'dve', 'pool', 'sp']
STAGES = "ABCDEFG"
DEBUG = None
LIM_BLK = None
LIM_PART = 9


class Prog:
    def __init__(self, nc):
        self.nc = nc
        self.ops = {e: [] for e in ENGS}
        self.cnt = {e: 0 for e in ENGS}
        self.seen = {e: {} for e in ENGS}
        self.lastw = {}
        self.readers = {}
        self.dcnt = {e: 0 for e in ENGS}
        self.NDS = 6
        self.pending = {e: {} for e in ENGS}
        self.semnames = set(ENGS)

    def _add(self, eng, fn, r, w, dma):
        waits = dict(self.pending[eng])
        self.pending[eng] = {}
        psr = [k for k in r if isinstance(k, str) and k.startswith('ps')]
        if psr:
            w = list(w) + psr

        def need(tok):
            if tok is None:
                return
            s, v = tok
            if s == eng and eng == 'pe':
                return
            if waits.get(s, 0) < v:
                waits[s] = v
        for k in r:
            need(self.lastw.get(k))
        for k in w:
            need(self.lastw.get(k))
            for s, v in self.readers.get(k, {}).items():
                need((s, v))
        if dma:
            n = self.dcnt[eng]
            self.dcnt[eng] += 1
            sname = "%s_d%d" % (eng, n % self.NDS)
            self.semnames.add(sname)
            val = 16 * (n // self.NDS + 1)
            if n >= self.NDS:
                need((sname, val - 16))
            tok = (sname, val)
        else:
            self.cnt[eng] += 1
            tok = (eng, self.cnt[eng])
        wl = []
        for s, v in waits.items():
            if self.seen[eng].get(s, 0) < v:
                self.seen[eng][s] = v
                wl.append((s, v))
        self.ops[eng].append((wl, fn, tok, dma))
        for k in w:
            self.lastw[k] = tok
            self.readers[k] = {}
        for k in r:
            d = self.readers.setdefault(k, {})
            if d.get(tok[0], 0) < tok[1]:
                d[tok[0]] = tok[1]
        return tok

    def barrier(self):
        toks = {}
        for e in ENGS:
            if self.cnt[e]:
                toks[e] = self.cnt[e]
            for i in range(min(self.dcnt[e], self.NDS)):
                n_last = ((self.dcnt[e] - 1 - i) // self.NDS) * self.NDS + i
                toks["%s_d%d" % (e, i)] = 16 * (n_last // self.NDS + 1)
        for e in ENGS:
            for s, v in toks.items():
                if self.pending[e].get(s, 0) < v:
                    self.pending[e][s] = v
        self.lastw = {}
        self.readers = {}

    def final_wait(self, eng):
        self.barrier()
        wl = [(s, v) for s, v in self.pending[eng].items() if self.seen[eng].get(s, 0) < v]
        self.ops[eng].append((wl, None, None, False))

    def mm(self, out, lhsT, rhs, start=True, stop=True, r=(), w=()):
        return self._add('pe', lambda h: h.matmul(out, lhsT, rhs, start=start, stop=stop, skip_group_check=True), r, w, False)

    def tr(self, out, in_, ident, r=(), w=()):
        return self._add('pe', lambda h: h.transpose(out, in_, ident), r, w, False)

    def act(self, out, in_, func, bias=0.0, scale=1.0, accum=None, r=(), w=()):
        if accum is None:
            return self._add('act', lambda h: h.activation(out, in_, func, bias=bias, scale=scale), r, w, False)
        return self._add('act', lambda h: h.activation(out, in_, func, bias=bias, scale=scale, accum_out=accum), r, w, False)

    def ts(self, eng, out, in0, s1, s2, op0, op1=None, r=(), w=()):
        if op1 is None:
            return self._add(eng, lambda h: h.tensor_scalar(out, in0, s1, None, op0), r, w, False)
        return self._add(eng, lambda h: h.tensor_scalar(out, in0, s1, s2, op0, op1), r, w, False)

    def tt(self, eng, out, in0, in1, op, r=(), w=()):
        return self._add(eng, lambda h: h.tensor_tensor(out, in0, in1, op), r, w, False)

    def stt(self, eng, out, in0, sc, in1, op0, op1, r=(), w=()):
        return self._add(eng, lambda h: h.scalar_tensor_tensor(out, in0, sc, in1, op0, op1), r, w, False)

    def cp(self, eng, out, in_, r=(), w=()):
        if eng == 'act':
            return self._add(eng, lambda h: h.copy(out, in_), r, w, False)
        return self._add(eng, lambda h: h.tensor_copy(out, in_), r, w, False)

    def red(self, eng, out, in_, op, r=(), w=()):
        return self._add(eng, lambda h: h.tensor_reduce(out, in_, AX.X, op), r, w, False)

    def memset(self, eng, out, val, r=(), w=()):
        return self._add(eng, lambda h: h.memset(out, val), r, w, False)

    def dma(self, eng, out, in_, r=(), w=()):
        return self._add(eng, lambda h: h.dma_start(out=out, in_=in_), r, w, True)

    def idma(self, out, out_off, in_, in_off, bc, r=(), w=()):
        return self._add('pool', lambda h: h.indirect_dma_start(
            out=out, out_offset=out_off, in_=in_, in_offset=in_off), r, w, True)

    def emit(self, block, sems):
        hmap = {'pe': 'tensor', 'act': 'scalar', 'dve': 'vector', 'pool': 'gpsimd', 'sp': 'sync'}
        for e in ENGS:
            ops = self.ops[e]
            if not ops:
                continue

            def body(h, ops=ops):
                for wl, fn, tok, dma in ops:
                    for s, v in wl:
                        h.wait_ge(sems[s], v)
                    if fn is not None:
                        fn(h).then_inc(sems[tok[0]], 16 if dma else 1)
            getattr(block, hmap[e])(body)


def bc_free(ap2d, n):
    a = list(ap2d.ap)
    return bass.AP(ap2d.tensor, ap2d.offset, [list(a[0]), list(a[1]), [0, n]])


def bc_mid(ap2d, n):
    a = list(ap2d.ap)
    return bass.AP(ap2d.tensor, ap2d.offset, [list(a[0]), [0, n], list(a[1])])


def build_program():
    nc = bass.Bass("TRN2", target_bir_lowering=False)
    P = Prog(nc)

    def din(name, shape, dt=F32):
        return nc.dram_tensor(name, list(shape), dt, kind="ExternalInput").ap()

    def dscr(name, shape, dt=F32):
        return nc.dram_tensor(name, list(shape), dt, kind="Internal").ap()

    xm = din("xm", [TOK, D])
    xp = din("xp", [PRE, D])
    pm = din("pm", [128, NBLK])
    hv = din("hv", [128, 1])
    cst = din("cst", [128, 4 * 128])
    w_in = din("w_in", [D, 11296])
    nmw = din("nmw", [128, 8])
    conv_w = din("conv_w", [128, 4, 20])
    conv_b = din("conv_b", [128, 20])
    dtb = din("dtb", [1, NH])
    alog = din("alog", [1, NH])
    dsk = din("dsk", [128, 16])
    snw = din("snw", [1, DI])
    w_ssm = din("w_ssm", [DI, D])
    qnw = din("qnw", [128, 1])
    knw = din("knw", [128, 1])
    biasT = din("biasT", [128, 24 * 2 * 128])
    w_attn = din("w_attn", [512, D])
    w_out = din("w_out", [D, D])
    nfw = din("nfw", [128, 8])
    w_rt = din("w_rt", [D, 72])
    b_rt = din("b_rt", [1, 72])
    nfw_row = din("nfw_row", [1, D])
    cst2 = din("cst2", [128, 320])
    tokid_d = din("tokid_d", [128, 16], I32)
    nE = NE if "F" in STAGES else 1
    w_g = din("w_g", [nE, D, 512])
    w_u = din("w_u", [nE, D, 512])
    w_d = din("w_d", [nE, 512, D])
    out = nc.dram_tensor("out", [TOK, D], F32, kind="ExternalOutput").ap()
    dbg = None
    if DEBUG is not None:
        dbg = nc.dram_tensor("dbg", list(DEBUG), F32, kind="ExternalOutput").ap()

    y_s = dscr("y_s", [TOK, DI])
    hT_s = dscr("hT_s", [2, 128, 8 * TOK], BF16)
    mg_s = dscr("mg_s", [TOK, D])
    xmid_s = dscr("xmid_s", [TOK, D])
    hn_s = dscr("hn_s", [TOK, D], BF16)
    idx_s = dscr("idx_s", [NE * CAP + 128, 1], I32)
    yrow_s = dscr("yrow_s", [NE * CAP + 128, D])

    w_in_v = w_in.rearrange("(kc p) n -> p kc n", p=128)

    from contextlib import ExitStack
    with ExitStack() as top:
        def sb(name, shape, dt=F32, stack=top):
            return stack.enter_context(nc.sbuf_tensor(name, list(shape), dt))

        def ps(name, shape, dt=F32, stack=top):
            return stack.enter_context(nc.psum_tensor(name, list(shape), dt))

        psF = [ps("psF%d" % i, [128, 512]) for i in range(6)]
        psB = [ps("psB%d" % i, [128, 1024], BF16) for i in range(2)]
        cstt = sb("cstt", [128, 512])
        identf, Umat, SLmat, onesf = cstt[:, 0:128], cstt[:, 128:256], cstt[:, 256:384], cstt[:, 384:512]
        identb = sb("identb", [128, 128], BF16)
        cstb = sb("cstb", [128, 384], BF16)
        Ub, SLb, onesb = cstb[:, 0:128], cstb[:, 128:256], cstb[:, 256:384]
        P.dma('sp', cstt[:], cst[:, :], w=['cst'])
        P.cp('dve', identb[:], identf, r=['cst'], w=['identb'])
        P.cp('dve', cstb[:], cstt[:, 128:512], r=['cst'], w=['cst'])

        if "A" in STAGES:
            with ExitStack() as st:
                Wx = sb("Wx", [128, 8, 2560], BF16, st)
                Wdt = sb("Wdt", [128, 8, 32], BF16, st)
                Rst = sb("Rst", [128, 2 * NH * 128], BF16, st)
                Rf = Rst[:].bitcast(F32)
                Rh = Rst[:, 0:NH * 128].rearrange("p (a b) -> p a b", a=NH)
                Rl = Rst[:, NH * 128:2 * NH * 128].rearrange("p (a b) -> p a b", a=NH)
                for kc in range(8):
                    P.dma('sp', Rf[:, 0:2048], w_in_v[:, kc, C_X:C_X + 2048], w=[('wst', 0)])
                    P.cp('pool', Wx[:, kc, 0:2048], Rf[:, 0:2048], r=[('wst', 0)], w=[('Wx', kc)])
                    P.dma('sp', Rf[:, 2048:2560], w_in_v[:, kc, C_X + 2048:C_X + 2560], w=[('wst', 1)])
                    P.cp('dve', Wx[:, kc, 2048:2560], Rf[:, 2048:2560], r=[('wst', 1)], w=[('Wx', kc)])
                P.dma('sp', Rf[:, 2560:2816].rearrange("p (a b) -> p a b", a=8), w_in_v[:, :, C_DT:C_DT + 32], w=[('wst', 2)])
                P.cp('dve', Wdt[:], Rf[:, 2560:2816].rearrange("p (a b) -> p a b", a=8), r=[('wst', 2)], w=['Wdt'])
                nmw_t = sb("nmw_t", [128, 8], F32, st)
                cw_t = sb("cw_t", [128, 4, 20], F32, st)
                cb_t = sb("cb_t", [128, 20], F32, st)
                dsk_t = sb("dsk_t", [128, 16], F32, st)
                pm_t = sb("pm_t", [128, NBLK], F32, st)
                dtb_t = sb("dtb_t", [128, NH], F32, st)
                a_t = sb("a_t", [128, NH], F32, st)
                P.dma('sp', nmw_t[:], nmw[:, :], w=['nmw'])
                P.dma('sp', cw_t[:], conv_w[:, :, :], w=['cw'])
                P.dma('sp', cb_t[:], conv_b[:, :], w=['cb'])
                P.dma('sp', dsk_t[:], dsk[:, :], w=['dsk'])
                P.dma('sp', pm_t[:], pm[:, :], w=['pm'])
                P.dma('sp', dtb_t[:], dtb[0].partition_broadcast(128), w=['dtb'])
                P.dma('sp', a_t[:], alog[0].partition_broadcast(128), w=['a'])
                P.act(a_t[:], a_t[:], AF.Exp, r=['a'], w=['a'])
                P.ts('dve', a_t[:], a_t[:], -1.0, None, ALU.mult, r=['a'], w=['a'])
                convdiag = sb("convdiag", [128, 4, 20, 128], BF16, st)
                for j in range(4):
                    for ct in range(20):
                        P.ts('dve', convdiag[:, j, ct, :], identf, cw_t[:, j, ct:ct + 1], None, ALU.mult,
                             r=['cst', 'cw'], w=[('cd', j, ct)])
                dskdiag = sb("dskdiag", [128, 16, 128], BF16, st)
                for ct in range(16):
                    P.ts('dve', dskdiag[:, ct, :], identf, dsk_t[:, ct:ct + 1], None, ALU.mult,
                         r=['cst', 'dsk'], w=[('dd', ct)])

                xt = [sb("xt%d" % i, [128, D], F32, st) for i in range(2)]
                xsq = sb("xsq", [128, D], F32, st)
                xn = sb("xn", [128, D], BF16, st)
                ss = sb("ss", [128, 2], F32, st)
                hT = sb("hT", [128, 8, 512], BF16, st)
                xin = sb("xin", [128, 20, 515], BF16, st)
                xbcT = sb("xbcT", [128, 20, 512], BF16, st)
                sm = sb("sm", [128, 14, NH], F32, st)
                smb = sb("smb", [128, 2, NH], BF16, st)
                expD = sb("expD", [128, NH, 128], BF16, st)
                MT = sb("MT", [128, NH, 128], BF16, st)
                cbm = sb("cbm", [128, 2, 128], BF16, st)
                xdt = sb("xdt", [128, DI], BF16, st)
                xw = sb("xw", [128, DI], BF16, st)
                Btok = sb("Btok", [128, 256], BF16, st)
                S = sb("S", [128, 2, 1024], F32, st)
                Sb = sb("Sb", [128, 2, 1024], BF16, st)
                ydg = sb("ydg", [128, DI], F32, st)
                yo = sb("yo", [128, DI], F32, st)
                P.memset('dve', S[:], 0.0, w=['S'])
                P.memset('dve', xin[:], 0.0, w=[('xin', c) for c in range(20)])
                tcount = 0
                for b in range(NBLK):
                    if LIM_BLK is not None and b not in LIM_BLK:
                        continue
                    main = b >= 12
                    src = xm if main else xp
                    row0 = (b - 12) * 512 if main else b * 512
                    nct = 20 if b >= 11 else 18
                    for t in range(4):
                        xb = xt[tcount % 2]
                        kx = ('xt', tcount % 2)
                        tcount += 1
                        P.dma('sp', xb[:], src[row0 + t * 128: row0 + (t + 1) * 128, :], w=[kx])
                        P.tt('pool', xsq[:], xb[:], xb[:], ALU.mult, r=[kx], w=['xsq'])
                        P.red('dve', ss[:, 0:1], xsq[:], ALU.add, r=['xsq'], w=['ss'])
                        P.ts('dve', ss[:, 1:2], ss[:, 0:1], 1.0 / D, EPS, ALU.mult, ALU.add, r=['ss'], w=['ss1'])
                        P.act(ss[:, 1:2], ss[:, 1:2], AF.Sqrt, r=['ss1'], w=['ss1'])
                        P._add('dve', lambda h, a=ss: h.reciprocal(a[:, 1:2], a[:, 1:2]), ['ss1'], ['ss1'], False)
                        P._add('act', lambda h, o=xn, i=xb, a=ss: h.mul(o[:], i[:], a[:, 1:2]), [kx, 'ss1'], ['xn'], False)
                        for kc in range(8):
                            P.tr(psB[0][:, kc * 128:(kc + 1) * 128], xn[:, kc * 128:(kc + 1) * 128], identb[:],
                                 r=['xn', 'identb'], w=['psB0'])
                        P.tt('dve', hT[:, :, t * 128:(t + 1) * 128], psB[0][:, :].rearrange("p (a b) -> p a b", a=8),
                             bc_free(nmw_t[:, :], 128), ALU.mult, r=['psB0', 'nmw'], w=[('hT', t)])
                    hkeys = [('hT', t) for t in range(4)]
                    if b >= 8:
                        seg = 1 if main else 0
                        c0 = ((b - 8) % 4) * 512
                        for kc in range(8):
                            P.dma('sp', hT_s[seg, :, kc * TOK + c0: kc * TOK + c0 + 512], hT[:, kc, :], r=hkeys)
                    if LIM_PART < 1:
                        continue
                    if b > 0:
                        P.cp('pool', xin[:, :, 0:3], xin[:, :, 512:515], r=[('xin', c) for c in range(20)],
                             w=[('xin', c) for c in range(20)])
                    for ct in range(nct):
                        pa, pb = psF[2 + 2 * (ct % 2)], psF[3 + 2 * (ct % 2)]
                        ka, kb = 'psF%d' % (2 + 2 * (ct % 2)), 'psF%d' % (3 + 2 * (ct % 2))
                        for kc in range(8):
                            P.mm(pa[:, :], Wx[:, kc, ct * 128:(ct + 1) * 128], hT[:, kc, :], start=(kc == 0), stop=(kc == 7),
                                 r=[('Wx', kc)] + hkeys, w=[ka])
                        P.cp('act', xin[:, ct, 3:515], pa[:, :], r=[ka], w=[('xin', ct)])
                        for j in range(4):
                            P.mm(pb[:, :], convdiag[:, j, ct, :], xin[:, ct, j:j + 512], start=(j == 0), stop=(j == 3),
                                 r=[('cd', j, ct), ('xin', ct)], w=[kb])
                        P.act(xbcT[:, ct, :], pb[:, :], AF.Silu, bias=cb_t[:, ct:ct + 1], r=[kb, 'cb'], w=[('xbc', ct)])
                    if LIM_PART == 1 and DEBUG is not None:
                        P.dma('sp', dbg[0:128, 0:2], ss[:, :], r=['ss', 'ss1'])
                        P.dma('sp', dbg[0:128, 8:16], nmw_t[:, :], r=['nmw'])
                        P.cp('dve', yo[:, 0:512], hT[:, 0, :], r=hkeys, w=['yo'])
                        P.cp('dve', yo[:, 512:1024], Wx[:, 0, 0:512], r=[('Wx', 0)], w=['yo'])
                        P.cp('dve', yo[:, 1024:1536], xin[:, 0, 3:515], r=[('xin', 0)], w=['yo'])
                        P.cp('dve', yo[:, 1536:1664], convdiag[:, 0, 0, :], r=[('cd', 0, 0)], w=['yo'])
                        P.cp('dve', yo[:, 1664:1792], identb[:], r=['identb'], w=['yo'])
                        P.cp('dve', yo[:, 1792:2048], xn[:, 0:256], r=['xn'], w=['yo'])
                        P.dma('sp', dbg[0:128, 16:16 + 2032], yo[:, 0:2032], r=['yo'])
                        for ct in range(16):
                            P.cp('dve', ydg[:, 0:512], xbcT[:, ct, :], r=[('xbc', ct)], w=['ydg'])
                            P.dma('sp', dbg[128 + ct * 128:128 + (ct + 1) * 128, 0:512], ydg[:, 0:512], r=['ydg'])
                    for t in range(4):
                        if LIM_PART < 2:
                            continue
                        cs_ = slice(t * 128, (t + 1) * 128)
                        dtr, av, ex, ln, dtv, adt, cs, cst_, dfs, dte, dtot, dtw = [sm[:, i, :] for i in range(12)]
                        for kc in range(8):
                            P.mm(psF[0][:, 0:32], hT[:, kc, cs_], Wdt[:, kc, :], start=(kc == 0), stop=(kc == 7),
                                 r=[('hT', t), 'Wdt'], w=['psF0'])
                        P.tt('dve', dtr, psF[0][:, 0:32], dtb_t[:], ALU.add, r=['psF0', 'dtb'], w=['dtr'])
                        P.act(ex, dtr, AF.Exp, r=['dtr'], w=['ex'])
                        P.act(dtv, ex, AF.Ln, bias=1.0, r=['ex'], w=['dtv'])
                        if not main:
                            P.ts('dve', dtv, dtv, pm_t[:, b:b + 1], None, ALU.mult, r=['dtv', 'pm'], w=['dtv'])
                        P.tt('dve', adt, dtv, a_t[:], ALU.mult, r=['dtv', 'a'], w=['adt'])
                        ahi, alo = smb[:, 0, :], smb[:, 1, :]
                        ahf, alf = sm[:, 12, :], sm[:, 13, :]
                        P.cp('dve', ahi, adt, r=['adt'], w=['ahi'])
                        P.cp('dve', ahf, ahi, r=['ahi'], w=['ahf'])
                        P.tt('dve', alf, adt, ahf, ALU.subtract, r=['adt', 'ahf'], w=['alf'])
                        P.cp('dve', alo, alf, r=['alf'], w=['alo'])
                        P.mm(psF[0][:, 32:64], Ub, ahi, start=True, stop=False, r=['cst', 'ahi'], w=['psF0'])
                        P.mm(psF[0][:, 32:64], Ub, alo, start=False, stop=True, r=['cst', 'alo'], w=['psF0'])
                        P.mm(psF[0][:, 64:96], onesb, ahi, start=True, stop=False, r=['cst', 'ahi'], w=['psF0'])
                        P.mm(psF[0][:, 64:96], onesb, alo, start=False, stop=True, r=['cst', 'alo'], w=['psF0'])
                        P.cp('dve', cs, psF[0][:, 32:64], r=['psF0'], w=['cs'])
                        P.act(dfs, psF[0][:, 32:64], AF.Exp, r=['psF0'], w=['dfs'])
                        P.act(dtot, psF[0][:, 64:96], AF.Exp, r=['psF0'], w=['dtot'])
                        P.tt('dve', dte, psF[0][:, 64:96], cs, ALU.subtract, r=['psF0', 'cs'], w=['dte'])
                        P.act(dte, dte, AF.Exp, r=['dte'], w=['dte'])
                        P.tt('dve', dtw, dte, dtv, ALU.mult, r=['dte', 'dtv'], w=['dtw'])
                        for half in range(2):
                            for c8 in range(8):
                                ct = half * 8 + c8
                                P.tr(psB[0][:, c8 * 128:(c8 + 1) * 128], xbcT[:, ct, cs_], identb[:],
                                     r=[('xbc', ct), 'identb'], w=['psB0'])
                            hs = slice(half * 1024, (half + 1) * 1024)
                            pv = psB[0][:, :].rearrange("p (h d) -> p h d", h=16)
                            hh = slice(half * 16, (half + 1) * 16)
                            P.tt('dve', xw[:, hs].rearrange("p (h d) -> p h d", h=16), pv, bc_free(dtw[:, hh], 64), ALU.mult,
                                 r=['psB0', 'dtw'], w=[('xw', half)])
                            if main:
                                P.tt('dve', xdt[:, hs].rearrange("p (h d) -> p h d", h=16), pv, bc_free(dtv[:, hh], 64),
                                     ALU.mult, r=['psB0', 'dtv'], w=[('xdt', half)])
                        for g in range(2):
                            P.tr(psB[1][:, g * 128:(g + 1) * 128], xbcT[:, 16 + g, cs_], identb[:],
                                 r=[('xbc', 16 + g), 'identb'], w=['psB1'])
                        P.cp('dve', Btok[:], psB[1][:, 0:256], r=['psB1'], w=['Btok'])
                        if main:
                            P.tt('pool', Rh, bc_mid(Ub, NH), bc_free(ahi, 128), ALU.mult, r=['cst', 'ahi'], w=['Rm', ('wst', 0), ('wst', 1), ('wst', 2)])
                            P.tt('pool', Rl, bc_mid(Ub, NH), bc_free(alo, 128), ALU.mult, r=['cst', 'alo'], w=['Rm', ('wst', 0), ('wst', 1), ('wst', 2)])
                            for g in range(2):
                                P.mm(psF[0][:, 128 + g * 128: 256 + g * 128], xbcT[:, 16 + g, cs_], xbcT[:, 18 + g, cs_],
                                     r=[('xbc', 16 + g), ('xbc', 18 + g)], w=['psF0'])
                            P.tt('dve', cbm[:], psF[0][:, 128:384].rearrange("p (g l) -> p g l", g=2), bc_mid(Umat, 2),
                                 ALU.mult, r=['psF0', 'cst'], w=['cbm'])
                            for h4 in range(8):
                                P.mm(psF[1][:, :], SLb, Rh[:, h4 * 4:(h4 + 1) * 4, :], start=True, stop=False, r=['cst', 'Rm'], w=['psF1'])
                                P.mm(psF[1][:, :], SLb, Rl[:, h4 * 4:(h4 + 1) * 4, :], start=False, stop=True, r=['cst', 'Rm'], w=['psF1'])
                                P.act(expD[:, h4 * 4:(h4 + 1) * 4, :], psF[1][:, :].rearrange("p (h l) -> p h l", h=4), AF.Exp,
                                      r=['psF1'], w=[('expD', h4)])
                            for g in range(2):
                                P.tt('dve', MT[:, g * 16:(g + 1) * 16, :], expD[:, g * 16:(g + 1) * 16, :],
                                     bc_mid(cbm[:, g, :], 16), ALU.mult,
                                     r=[('expD', h4) for h4 in range(g * 4, g * 4 + 4)] + ['cbm'], w=[('MT', g)])
                            P.cp('act', Sb[:], S[:], r=['S'], w=['Sb'])
                            for g in range(2):
                                for j in range(2):
                                    kq = 'psF%d' % (4 + j)
                                    P.mm(psF[4 + j][:, :], xbcT[:, 18 + g, cs_], Sb[:, g, j * 512:(j + 1) * 512],
                                         r=[('xbc', 18 + g), 'Sb'], w=[kq])
                                    P.cp('act', yo[:, g * 1024 + j * 512: g * 1024 + (j + 1) * 512], psF[4 + j][:, :],
                                         r=[kq], w=[('yo', g, j)])
                                for j in range(2):
                                    kq = 'psF%d' % (2 + j)
                                    for c4 in range(4):
                                        ct = g * 8 + j * 4 + c4
                                        P.mm(psF[2 + j][:, c4 * 128:(c4 + 1) * 128], xbcT[:, ct, cs_], dskdiag[:, ct, :],
                                             start=True, stop=False, r=[('xbc', ct), ('dd', ct)], w=[kq])
                                        for hh2 in range(2):
                                            h = ct * 2 + hh2
                                            P.mm(psF[2 + j][:, c4 * 128 + hh2 * 64: c4 * 128 + (hh2 + 1) * 64], MT[:, h, :],
                                                 xdt[:, h * 64:(h + 1) * 64], start=False, stop=True,
                                                 r=[('MT', g), ('xdt', h // 16)], w=[kq])
                                    for h8 in range(8):
                                        h = g * 16 + j * 8 + h8
                                        P.stt('dve', ydg[:, h * 64:(h + 1) * 64], yo[:, h * 64:(h + 1) * 64], dfs[:, h:h + 1],
                                              psF[2 + j][:, h8 * 64:(h8 + 1) * 64], ALU.mult, ALU.add,
                                              r=[('yo', g, j), 'dfs', kq], w=['ydg'])
                            tok0 = (b - 12) * 512 + t * 128
                            P.dma('sp', y_s[tok0:tok0 + 128, :], ydg[:], r=['ydg'])
                            if DEBUG is not None and STAGES == "A":
                                P.dma('sp', dbg[128 + tok0:128 + tok0 + 128, :], ydg[:], r=['ydg'])
                        for g in range(2):
                            P.tt('pool', S[:, g, :].rearrange("p (h d) -> p h d", h=16), S[:, g, :].rearrange("p (h d) -> p h d", h=16),
                                 bc_free(dtot[:, g * 16:(g + 1) * 16], 64), ALU.mult, r=['S', 'dtot', 'Sb'], w=['S'])
                            for j in range(2):
                                kq = 'psF%d' % (4 + j)
                                P.mm(psF[4 + j][:, :], Btok[:, g * 128:(g + 1) * 128], xw[:, g * 1024 + j * 512: g * 1024 + (j + 1) * 512],
                                     r=['Btok', ('xw', g)], w=[kq])
                                P.tt('dve', S[:, g, j * 512:(j + 1) * 512], S[:, g, j * 512:(j + 1) * 512], psF[4 + j][:, :], ALU.add,
                                     r=['S', kq], w=['S'])
                if DEBUG is not None and STAGES == "A" and LIM_PART > 1:
                    P.dma('sp', dbg[0:128, 0:2048], S[:].rearrange("p g c -> p (g c)"), r=['S'])
                P.barrier()

        def wload(dst, srcfn, nk, ncols, stg, name):
            for kc in range(nk):
                sgt = stg[kc % 2]
                P.dma('sp', sgt[:, 0:ncols], srcfn(kc), w=[('stg', sgt.name if hasattr(sgt, 'name') else id(sgt))])
                P.cp('dve' if kc % 2 == 0 else 'pool', dst[:, kc, :], sgt[:, 0:ncols],
                     r=[('stg', sgt.name if hasattr(sgt, 'name') else id(sgt))], w=[(name, kc)])

        def strided(t, part, s0, r, n):
            base = t[part, s0:s0 + 1]
            a = list(base.ap)
            return bass.AP(base.tensor, base.offset, [list(a[0]), [r, n]])

        def rstd_ops(dst, src_sum, scale, keyr, keyw):
            P.ts('dve', dst, src_sum, scale, EPS, ALU.mult, ALU.add, r=keyr, w=keyw)
            P.act(dst, dst, AF.Sqrt, r=keyw, w=keyw)
            P._add('dve', lambda h, a=dst: h.reciprocal(a, a), keyw, keyw, False)

        aoT = sb("aoT", [128, 4, TOK], BF16)
        ohA = sb("ohA", [128, 16, 64])
        ohB = sb("ohB", [128, 16, 64])
        gts = sb("gts", [128, 16, 2])
        dstA = sb("dstA", [128, 16], I32)
        dstB = sb("dstB", [128, 16], I32)
        cst2t = sb("cst2t", [128, 320])
        SUmat, blk1, iotae = cst2t[:, 0:128], cst2t[:, 128:256], cst2t[:, 256:320]
        tokid = sb("tokid", [128, 16], I32)
        P.dma('sp', cst2t[:], cst2[:, :], w=['cst2'])
        P.dma('sp', tokid[:], tokid_d[:, :], w=['tokid'])
        w_ssm_v = w_ssm.rearrange("(c p) n -> p c n", p=128)
        w_attn_v = w_attn.rearrange("(c p) n -> p c n", p=128)
        w_out_v = w_out.rearrange("(c p) n -> p c n", p=128)
        w_rt_v = w_rt.rearrange("(c p) n -> p c n", p=128)

        if "B" in STAGES:
            with ExitStack() as st:
                stg = [sb("stgB%d" % i, [128, 2048], F32, st) for i in range(2)]
                Wz = sb("Wz", [128, 8, 2048], BF16, st)
                Wss = sb("Wss", [128, 16, 1024], BF16, st)
                Wgs = sb("Wgs", [128, 8, 1024], BF16, st)
                wload(Wz, lambda kc: w_in_v[:, kc, C_Z:C_Z + 2048], 8, 2048, stg, 'Wz')
                wload(Wss, lambda c: w_ssm_v[:, c, :], 16, 1024, stg, 'Wss')
                wload(Wgs, lambda kc: w_in_v[:, kc, C_GS:C_GS + 1024], 8, 1024, stg, 'Wgs')
                snw_t = sb("snw_t", [128, DI], F32, st)
                P.dma('sp', snw_t[:], snw[0].partition_broadcast(128), w=['snw'])
                hTm = sb("hTm", [128, 8, TOK], BF16, st)
                for kc in range(8):
                    P.dma('sp', hTm[:, kc, :], hT_s[1, :, kc * TOK:(kc + 1) * TOK], w=[('hTm', kc)])
                hk = [('hTm', kc) for kc in range(8)]
                yt = sb("ytB", [128, DI], F32, st)
                zs = sb("zsB", [128, DI], F32, st)
                y5 = yt
                sq = zs
                y6 = sb("y6B", [128, DI], BF16, st)
                yT = sb("yTB", [128, 16, 128], BF16, st)
                sg = sb("sgB", [128, D], F32, st)
                mo = sb("moB", [128, D], F32, st)
                ssb = sb("ssB", [128, 4], F32, st)
                for i in range(16):
                    ts_ = slice(i * 128, (i + 1) * 128)
                    P.dma('sp', yt[:], y_s[ts_, :], w=['yt'])
                    for cb in range(4):
                        kq = 'psF%d' % cb
                        for kc in range(8):
                            P.mm(psF[cb][:, :], hTm[:, kc, ts_], Wz[:, kc, cb * 512:(cb + 1) * 512], start=(kc == 0), stop=(kc == 7),
                                 r=[('hTm', kc), ('Wz', kc)], w=[kq])
                        P.act(zs[:, cb * 512:(cb + 1) * 512], psF[cb][:, :], AF.Silu, r=[kq], w=[('zs', cb)])
                    zk = [('zs', cb) for cb in range(4)]
                    P.tt('dve', y5[:], yt[:], zs[:], ALU.mult, r=['yt'] + zk, w=['y5', 'yt'])
                    P.tt('pool', sq[:], y5[:], y5[:], ALU.mult, r=['y5', 'yt'], w=['sq'] + zk)
                    P.red('dve', ssb[:, 0:2], sq[:].rearrange("p (g c) -> p g c", g=2), ALU.add, r=['sq'] + zk, w=['ssb0'])
                    rstd_ops(ssb[:, 2:4], ssb[:, 0:2], 1.0 / 1024, ['ssb0'], ['ssb1'])
                    for g in range(2):
                        gs_ = slice(g * 1024, (g + 1) * 1024)
                        P.stt('dve', y6[:, gs_], y5[:, gs_], ssb[:, 2 + g:3 + g], snw_t[:, gs_], ALU.mult, ALU.mult,
                              r=['y5', 'yt', 'ssb1', 'snw'], w=[('y6', g)])
                    for half in range(2):
                        for c8 in range(8):
                            c = half * 8 + c8
                            P.tr(psB[half][:, c8 * 128:(c8 + 1) * 128], y6[:, c * 128:(c + 1) * 128], identb[:],
                                 r=[('y6', c // 8), 'identb'], w=['psB%d' % half])
                        P.cp('dve' if half == 0 else 'act', yT[:, half * 8:(half + 1) * 8, :],
                             psB[half][:, :].rearrange("p (a b) -> p a b", a=8), r=['psB%d' % half], w=[('yT', half)])
                    for cb in range(2):
                        kq = 'psF%d' % (4 + cb)
                        for c in range(16):
                            P.mm(psF[4 + cb][:, :], yT[:, c, :], Wss[:, c, cb * 512:(cb + 1) * 512], start=(c == 0), stop=(c == 15),
                                 r=[('yT', c // 8), ('Wss', c)], w=[kq])
                    for cb in range(2):
                        kq = 'psF%d' % cb
                        for kc in range(8):
                            P.mm(psF[cb][:, :], hTm[:, kc, ts_], Wgs[:, kc, cb * 512:(cb + 1) * 512], start=(kc == 0), stop=(kc == 7),
                                 r=[('hTm', kc), ('Wgs', kc)], w=[kq])
                        P.act(sg[:, cb * 512:(cb + 1) * 512], psF[cb][:, :], AF.Sigmoid, r=[kq], w=[('sg', cb)])
                        P.tt('dve', mo[:, cb * 512:(cb + 1) * 512], sg[:, cb * 512:(cb + 1) * 512], psF[4 + cb][:, :], ALU.mult,
                             r=[('sg', cb), 'psF%d' % (4 + cb)], w=[('mo', cb)])
                    P.dma('sp', mg_s[ts_, :], mo[:], r=[('mo', 0), ('mo', 1)])
                    if DEBUG is not None and STAGES[-1] == "B":
                        P.dma('sp', dbg[ts_, 0:1024], mo[:], r=[('mo', 0), ('mo', 1)])
                P.barrier()

        if "C" in STAGES:
            with ExitStack() as st:
                hTa = sb("hTa", [128, 8, 2 * TOK], BF16, st)
                for kc in range(8):
                    P.dma('sp', hTa[:, kc, 0:TOK], hT_s[0, :, kc * TOK:(kc + 1) * TOK], w=[('hTa', kc)])
                    P.dma('sp', hTa[:, kc, TOK:2 * TOK], hT_s[1, :, kc * TOK:(kc + 1) * TOK], w=[('hTa', kc)])
                stg = [sb("stgC%d" % i, [128, 1024], F32, st) for i in range(2)]
                biasb = sb("biasb", [128, 6, 1024], BF16, st)
                wload(biasb, lambda c: biasT[:, c * 1024:(c + 1) * 1024], 6, 1024, stg, 'biasb')
                biasv = biasb[:].rearrange("p c (a q) -> p (c a) q", q=128)
                hv_t = sb("hv_t", [128, 1], F32, st)
                qn_t = sb("qn_t", [128, 1], F32, st)
                kn_t = sb("kn_t", [128, 1], F32, st)
                P.dma('sp', hv_t[:], hv[:, :], w=['hv'])
                P.dma('sp', qn_t[:], qnw[:, :], w=['qn'])
                P.dma('sp', kn_t[:], knw[:, :], w=['kn'])
                P.ts('dve', qn_t[:], qn_t[:], 0.125, None, ALU.mult, r=['qn'], w=['qn'])
                knAB = sb("knAB", [128, 2], F32, st)
                P.memset('dve', knAB[:], 0.0, w=['knAB'])
                P.cp('dve', knAB[0:64, 0:1], kn_t[0:64, 0:1], r=['kn'], w=['knAB'])
                P.cp('dve', knAB[64:128, 1:2], kn_t[64:128, 0:1], r=['kn'], w=['knAB'])
                KTB = sb("KTB", [128, 2 * TOK], BF16, st)
                Emat = sb("Emat", [128, 4, 128], BF16, st)
                P.memset('dve', Emat[:], 0.0, w=['E'])
                P.memset('dve', Emat[:, 0, 0:64], 1.0, w=['E'])
                P.memset('dve', Emat[:, 1, 64:128], 1.0, w=['E'])
                P.ts('dve', Emat[:, 2:4, :], Emat[:, 0:2, :], hv_t[:, 0:1], None, ALU.mult, r=['E', 'hv'], w=['E'])
                Wq = sb("WqC", [128, 8, 128], BF16, st)
                Wk = sb("WkC", [128, 8, 128], BF16, st)
                Wv = sb("WvC", [128, 8, 128], BF16, st)
                KT = sb("KT", [128, 2 * TOK], BF16, st)
                QT = sb("QT", [128, TOK], BF16, st)
                qs = sb("qsC", [128, 512], F32, st)
                sq = sb("sqC", [128, 512], BF16, st)
                blkb = sb("blkb", [128, 128], BF16, st)
                P.cp('dve', blkb[:], blk1, r=['cst2'], w=['blkb'])
                rs = sb("rsC", [128, 512], F32, st)
                VA = sb("VA", [128, 32, 128], BF16, st)
                VB = sb("VB", [128, 32, 128], BF16, st)
                P.memset('pool', VA[:], 0.0, w=['VA'])
                P.memset('pool', VB[:], 0.0, w=['VB'])
                PT = sb("PT", [128, 512], BF16, st)
                acc = sb("accC", [128, 2, TOK], F32, st)
                hka = [('hTa', kc) for kc in range(8)]

                def proj_norm(Wt, wname, tok0, w_ap, dst, keyw, dst2=None, w_ap2=None):
                    for kc in range(8):
                        P.mm(psF[0][:, :], Wt[:, kc, :], hTa[:, kc, tok0:tok0 + 512], start=(kc == 0), stop=(kc == 7),
                             r=[(wname, kc), ('hTa', kc)], w=['psF0'])
                    P.cp('act', qs[:], psF[0][:, :], r=['psF0'], w=['qs'])
                    P.tt('pool', sq[:], qs[:], qs[:], ALU.mult, r=['qs'], w=['sq'])
                    P.mm(psF[1][:, :], blkb[:], sq[:], r=['blkb', 'sq'], w=['psF1'])
                    rstd_ops(rs[:], psF[1][:, :], 1.0 / 64, ['psF1'], ['rs'])
                    P.stt('dve', dst, qs[:], w_ap, rs[:], ALU.mult, ALU.mult, r=['qs', 'rs', 'qn', 'kn', 'knAB'], w=[keyw])
                    if dst2 is not None:
                        P.stt('dve', dst2, qs[:], w_ap2, rs[:], ALU.mult, ALU.mult, r=['qs', 'rs', 'knAB'], w=[keyw])

                for hp in range(4):
                    for g, r_ in enumerate((1, 4, 16)):
                        hcol = (g * 8 + hp * 2) * 64
                        for Wt, wn, c0 in ((Wq, 'Wq', C_Q), (Wk, 'Wk', C_K), (Wv, 'Wv', C_V)):
                            sgt = stg[0] if wn != 'Wk' else stg[1]
                            skey = ('stg', sgt.name if hasattr(sgt, 'name') else id(sgt))
                            P.dma('sp', sgt[:, :].rearrange("p (a b) -> p a b", a=8), w_in_v[:, :, c0 + hcol:c0 + hcol + 128], w=[skey])
                            P.cp('dve', Wt[:], sgt[:, :].rearrange("p (a b) -> p a b", a=8), r=[skey], w=[(wn, kc) for kc in range(8)])
                        for tb in range(4):
                            proj_norm(Wq, 'Wq', TOK + tb * 512, qn_t[:, 0:1], QT[:, tb * 512:(tb + 1) * 512], 'QT')
                        for tb in range(8):
                            proj_norm(Wk, 'Wk', tb * 512, knAB[:, 0:1], KT[:, tb * 512:(tb + 1) * 512], 'KT',
                                      KTB[:, tb * 512:(tb + 1) * 512], knAB[:, 1:2])
                        nbk = 32 // r_
                        for rho in range(r_):
                            for jb in range(nbk):
                                ti = rho * nbk + jb
                                s0 = rho + r_ * 128 * jb
                                for kc in range(8):
                                    lh = hTa[:, kc, s0:s0 + 1]
                                    a_ = list(lh.ap)
                                    lhs = bass.AP(lh.tensor, lh.offset, [list(a_[0]), [r_, 128]])
                                    P.mm(psF[2][:, 0:128], lhs, Wv[:, kc, :], start=(kc == 0), stop=(kc == 7),
                                         r=[('hTa', kc), ('Wv', kc)], w=['psF2'])
                                if jb < nbk // 2:
                                    P.ts('dve', VA[:, ti, 0:64], psF[2][:, 0:64], hv_t[:, 0:1], None, ALU.mult, r=['psF2', 'hv'], w=['VA'])
                                    P.ts('dve', VB[:, ti, 64:128], psF[2][:, 64:128], hv_t[:, 0:1], None, ALU.mult, r=['psF2', 'hv'], w=['VB'])
                                else:
                                    P.cp('dve', VA[:, ti, 0:64], psF[2][:, 0:64], r=['psF2'], w=['VA'])
                                    P.cp('act', VB[:, ti, 64:128], psF[2][:, 64:128], r=['psF2'], w=['VB'])
                        for rho in range(r_):
                            for n in range(16 // r_):
                                jbp = 16 // r_ + n - 1
                                tp = rho * nbk + jbp
                                qsl = lambda ph: strided(QT, ph, rho + r_ * 128 * n, r_, 128)
                                bank = 3 + (n + rho) % 2
                                kS = 'psF%d' % bank
                                for hh in range(2):
                                    ph = slice(0, 128)
                                    h = g * 8 + hp * 2 + hh
                                    for pc in range(2):
                                        col = (hh * 2 + pc) * 128
                                        ksl = strided(KT if hh == 0 else KTB, ph, rho + r_ * 128 * (jbp + pc), r_, 128)
                                        P.mm(psF[bank][:, col:col + 128], ksl, qsl(ph), start=True, stop=False,
                                             r=['KT', 'QT'], w=[kS])
                                        P.mm(psF[bank][:, col:col + 128], identb[:], biasv[:, h * 2 + pc, :], start=False, stop=True,
                                             r=['identb'] + [('biasb', c) for c in range(6)], w=[kS])
                                P.act(PT[:], psF[bank][:, :], AF.Exp, r=[kS], w=['PT'])
                                for q4, (Vt, tix) in enumerate(((VA, tp), (VA, tp + 1), (VB, tp), (VB, tp + 1))):
                                    P.mm(psF[5][:, 0:128], Vt[:, tix, :], PT[:, q4 * 128:(q4 + 1) * 128], start=(q4 == 0), stop=(q4 == 3),
                                         r=['VA', 'VB', 'PT'], w=['psF5'])
                                for q4, ei in enumerate((2, 0, 3, 1) if n == 0 else (0, 0, 1, 1)):
                                    P.mm(psF[5][:, 128:256], Emat[:, ei, :], PT[:, q4 * 128:(q4 + 1) * 128], start=(q4 == 0), stop=(q4 == 3),
                                         r=['E', 'PT'], w=['psF5'])
                                q0 = rho + r_ * 128 * n
                                ab = acc[:, 0:1, q0:q0 + 1]
                                aa = list(ab.ap)
                                accv = bass.AP(ab.tensor, ab.offset, [list(aa[0]), [TOK, 2], [r_, 128]])
                                pv = psF[5][:, 0:256].rearrange("p (a q) -> p a q", a=2)
                                if g == 0:
                                    P.cp('dve', accv, pv, r=['psF5'], w=['acc'])
                                else:
                                    P.tt('dve', accv, accv, pv, ALU.add, r=['psF5', 'acc'], w=['acc'])
                    P._add('dve', lambda h, a=acc: h.reciprocal(a[:, 1, :], a[:, 1, :]), ['acc'], ['acc'], False)
                    P.tt('dve', aoT[:, hp, :], acc[:, 0, :], acc[:, 1, :], ALU.mult, r=['acc'], w=[('aoT', hp)])
                if DEBUG is not None and STAGES[-1] == "C":
                    for hp in range(4):
                        P.cp('dve', acc[:, 0, :], aoT[:, hp, :], r=[('aoT', hp)], w=['acc'])
                        P.dma('sp', dbg[hp * 128:(hp + 1) * 128, 0:TOK], acc[:, 0, :], r=['acc'])
                P.barrier()

        if "D" in STAGES:
            with ExitStack() as st:
                stg = [sb("stgD%d" % i, [128, 1024], F32, st) for i in range(2)]
                Wat = sb("Wat", [128, 4, 1024], BF16, st)
                Wga = sb("Wga", [128, 8, 1024], BF16, st)
                Wo = sb("Wo", [128, 8, 1024], BF16, st)
                Wr = sb("Wr", [128, 8, 72], BF16, st)
                wload(Wat, lambda c: w_attn_v[:, c, :], 4, 1024, stg, 'Wat')
                wload(Wga, lambda kc: w_in_v[:, kc, C_GA:C_GA + 1024], 8, 1024, stg, 'Wga')
                wload(Wo, lambda c: w_out_v[:, c, :], 8, 1024, stg, 'Wo')
                wload(Wr, lambda c: w_rt_v[:, c, :], 8, 72, stg, 'Wr')
                hTm = sb("hTmD", [128, 8, TOK], BF16, st)
                for kc in range(8):
                    P.dma('sp', hTm[:, kc, :], hT_s[1, :, kc * TOK:(kc + 1) * TOK], w=[('hTm', kc)])
                nfw_t = sb("nfw_t", [128, D], F32, st)
                brt_t = sb("brt_t", [128, 72], F32, st)
                P.dma('sp', nfw_t[:], nfw_row[0].partition_broadcast(128), w=['nfw'])
                P.dma('sp', brt_t[:], b_rt[0].partition_broadcast(128), w=['brt'])
                xt_ = sb("xtD", [128, D], F32, st)
                mgt = sb("mgtD", [128, D], F32, st)
                sg = sb("sgD", [128, D], F32, st)
                tmp = sb("tmpD", [128, D], F32, st)
                mrg = sb("mrgD", [128, D], BF16, st)
                mT = sb("mTD", [128, 8, 128], BF16, st)
                xmd = sb("xmdD", [128, D], F32, st)
                sq = sb("sqD", [128, D], F32, st)
                hn = sb("hnD", [128, D], BF16, st)
                hnT = sb("hnTD", [128, 8, 128], BF16, st)
                ssd = sb("ssD", [128, 2], F32, st)
                lg = sb("lgD", [128, 72], F32, st)
                rt = sb("rtD", [128, 16, 8], F32, st)
                f3 = sb("f3D", [128, 8, 8], F32, st)
                for i in range(16):
                    ts_ = slice(i * 128, (i + 1) * 128)
                    P.dma('sp', xt_[:], xm[ts_, :], w=['xt'])
                    P.dma('sp', mgt[:], mg_s[ts_, :], w=['mgt'])
                    for cb in range(2):
                        kq = 'psF%d' % cb
                        for hp in range(4):
                            P.mm(psF[cb][:, :], aoT[:, hp, ts_], Wat[:, hp, cb * 512:(cb + 1) * 512], start=(hp == 0), stop=(hp == 3),
                                 r=[('aoT', hp), ('Wat', hp)], w=[kq])
                        kg = 'psF%d' % (2 + cb)
                        for kc in range(8):
                            P.mm(psF[2 + cb][:, :], hTm[:, kc, ts_], Wga[:, kc, cb * 512:(cb + 1) * 512], start=(kc == 0), stop=(kc == 7),
                                 r=[('hTm', kc), ('Wga', kc)], w=[kg])
                        cs2 = slice(cb * 512, (cb + 1) * 512)
                        P.act(sg[:, cs2], psF[2 + cb][:, :], AF.Sigmoid, r=[kg], w=[('sg', cb)])
                        P.tt('dve', tmp[:, cs2], sg[:, cs2], psF[cb][:, :], ALU.mult, r=[('sg', cb), kq], w=[('tmp', cb)])
                        P.tt('pool', mrg[:, cs2], tmp[:, cs2], mgt[:, cs2], ALU.add, r=[('tmp', cb), 'mgt'], w=[('mrg', cb)])
                    for c in range(8):
                        P.tr(psB[0][:, c * 128:(c + 1) * 128], mrg[:, c * 128:(c + 1) * 128], identb[:],
                             r=[('mrg', c // 4), 'identb'], w=['psB0'])
                    P.cp('dve', mT[:], psB[0][:, :].rearrange("p (a b) -> p a b", a=8), r=['psB0'], w=['mT'])
                    for cb in range(2):
                        kq = 'psF%d' % (4 + cb)
                        cs2 = slice(cb * 512, (cb + 1) * 512)
                        for c in range(8):
                            P.mm(psF[4 + cb][:, :], mT[:, c, :], Wo[:, c, cs2], start=(c == 0), stop=(c == 7),
                                 r=['mT', ('Wo', c)], w=[kq])
                        P.tt('dve', xmd[:, cs2], xt_[:, cs2], psF[4 + cb][:, :], ALU.add, r=['xt', kq], w=[('xmd', cb)])
                    xk = [('xmd', 0), ('xmd', 1)]
                    P.dma('sp', xmid_s[ts_, :], xmd[:], r=xk)
                    if DEBUG is not None and STAGES[-1] == "D":
                        P.dma('sp', dbg[ts_, 0:1024], xmd[:], r=xk)
                    P.tt('pool', sq[:], xmd[:], xmd[:], ALU.mult, r=xk, w=['sq'])
                    P.red('dve', ssd[:, 0:1], sq[:], ALU.add, r=['sq'], w=['ssd0'])
                    rstd_ops(ssd[:, 1:2], ssd[:, 0:1], 1.0 / D, ['ssd0'], ['ssd1'])
                    P.stt('dve', hn[:], xmd[:], ssd[:, 1:2], nfw_t[:], ALU.mult, ALU.mult, r=xk + ['ssd1', 'nfw'], w=['hn'])
                    P.dma('sp', hn_s[ts_, :], hn[:], r=['hn'])
                    for c in range(8):
                        P.tr(psB[1][:, c * 128:(c + 1) * 128], hn[:, c * 128:(c + 1) * 128], identb[:], r=['hn', 'identb'], w=['psB1'])
                    P.cp('act', hnT[:], psB[1][:, :].rearrange("p (a b) -> p a b", a=8), r=['psB1'], w=['hnT'])
                    for kc in range(8):
                        P.mm(psF[0][:, 0:72], hnT[:, kc, :], Wr[:, kc, :], start=(kc == 0), stop=(kc == 7),
                             r=['hnT', ('Wr', kc)], w=['psF0'])
                    P.tt('dve', lg[:], psF[0][:, 0:72], brt_t[:], ALU.add, r=['psF0', 'brt'], w=['lg'])
                    m, negm, ohg, ex, se, gp, sel, m1, oh1, sel2, m2, oh2, dd, s1 = [rt[:, k, :] for k in range(14)]
                    K_ = ['lg', 'rt']
                    P.red('dve', m[:, 0:1], lg[:, 0:8], ALU.max, r=['lg'], w=['rt'])
                    P.ts('dve', negm[:, 0:1], m[:, 0:1], -1.0, None, ALU.mult, r=['rt'], w=['rt'])
                    P.ts('dve', ohg, lg[:, 0:8], m[:, 0:1], None, ALU.is_equal, r=K_, w=['rt'])
                    P.act(ex, lg[:, 0:8], AF.Exp, bias=negm[:, 0:1], r=K_, w=['rt'])
                    P.red('dve', se[:, 0:1], ex, ALU.add, r=['rt'], w=['rt'])
                    P._add('dve', lambda h, a=gp[:, 0:1], b_=se[:, 0:1]: h.reciprocal(a, b_), ['rt'], ['rt'], False)
                    P.tt('dve', f3[:], lg[:, 8:72].rearrange("p (g e) -> p g e", g=8), bc_free(ohg, 8), ALU.mult, r=K_, w=['f3'])
                    P.red('dve', sel, f3[:].rearrange("p g e -> p e g"), ALU.add, r=['f3'], w=['rt'])
                    P.red('dve', m1[:, 0:1], sel, ALU.max, r=['rt'], w=['rt'])
                    P.ts('dve', oh1, sel, m1[:, 0:1], None, ALU.is_equal, r=['rt'], w=['rt'])
                    P.stt('dve', sel2, oh1, -1.0e9, sel, ALU.mult, ALU.add, r=['rt'], w=['rt'])
                    P.red('dve', m2[:, 0:1], sel2, ALU.max, r=['rt'], w=['rt'])
                    P.ts('dve', oh2, sel2, m2[:, 0:1], None, ALU.is_equal, r=['rt'], w=['rt'])
                    P.tt('dve', dd[:, 0:1], m1[:, 0:1], m2[:, 0:1], ALU.subtract, r=['rt'], w=['rt'])
                    P.act(s1[:, 0:1], dd[:, 0:1], AF.Sigmoid, r=['rt'], w=['rt'])
                    P.tt('dve', gts[:, i, 0:1], gp[:, 0:1], s1[:, 0:1], ALU.mult, r=['rt'], w=['gts'])
                    P.tt('dve', gts[:, i, 1:2], gp[:, 0:1], gts[:, i, 0:1], ALU.subtract, r=['rt', 'gts'], w=['gts'])
                    P.tt('dve', ohA[:, i, :].rearrange("p (g e) -> p g e", g=8), bc_free(ohg, 8), bc_mid(oh1, 8), ALU.mult,
                         r=['rt'], w=['ohA'])
                    P.tt('dve', ohB[:, i, :].rearrange("p (g e) -> p g e", g=8), bc_free(ohg, 8), bc_mid(oh2, 8), ALU.mult,
                         r=['rt'], w=['ohB'])
                P.barrier()

        if "E" in STAGES:
            with ExitStack() as st:
                zi = sb("ziE", [128, 64], I32, st)
                P.memset('dve', zi[:], 0, w=['zi'])
                P.dma('sp', idx_s[0:NE * CAP, :].rearrange("(p c) o -> p (c o)", p=128), zi[:], r=['zi'], w=['idx_s'])
                zf = sb("zfE", [128, D], F32, st)
                P.memset('pool', zf[:], 0.0, w=['zf'])
                P.dma('sp', yrow_s[NE * CAP:NE * CAP + 128, :], zf[:], r=['zf'])
                Racc = sb("Racc", [128, 64], F32, st)
                Ms = sb("MsE", [128, 64], F32, st)
                slot = sb("slotE", [128, 64], F32, st)
                ov = sb("ovE", [128, 64], F32, st)
                t2 = sb("t2E", [128, 64], F32, st)
                df = sb("dfE", [128, 2], F32, st)
                P.memset('dve', Racc[:], 0.0, w=['Racc'])
                Msb = sb("MsbE", [128, 2, 64], BF16, st)
                SUb = sb("SUbE", [128, 128], BF16, st)
                P.cp('dve', SUb[:], SUmat, r=['cst2'], w=['SUb'])
                for i in range(16):
                    P.tt('dve', Ms[:], ohA[:, i, :], ohB[:, i, :], ALU.add, r=['ohA', 'ohB'], w=['Ms'])
                    P.cp('dve', Msb[:, 0, :], Ms[:], r=['Ms'], w=['Msb'])
                    P.cp('dve', Msb[:, 1, :], Racc[:], r=['Racc'], w=['Msb'])
                    P.mm(psF[0][:, 0:64], SUb[:], Msb[:, 0, :], start=True, stop=False, r=['SUb', 'Msb'], w=['psF0'])
                    P.mm(psF[0][:, 0:64], onesb, Msb[:, 1, :], start=False, stop=True, r=['cst', 'Msb'], w=['psF0'])
                    P.ts('dve', ov[:], psF[0][:, 0:64], float(CAP), 1.0e6, ALU.is_ge, ALU.mult, r=['psF0'], w=['ov'])
                    P.tt('dve', slot[:], psF[0][:, 0:64], iotae, ALU.add, r=['psF0', 'cst2'], w=['slot'])
                    P.tt('dve', slot[:], slot[:], ov[:], ALU.add, r=['slot', 'ov'], w=['slot'])
                    for k, (oh, dst) in enumerate(((ohA, dstA), (ohB, dstB))):
                        P.tt('dve', t2[:], oh[:, i, :], slot[:], ALU.mult, r=['ohA', 'ohB', 'slot'], w=['t2'])
                        P.red('dve', df[:, k:k + 1], t2[:], ALU.add, r=['t2'], w=['df'])
                        P.ts('dve', df[:, k:k + 1], df[:, k:k + 1], float(NE * CAP), None, ALU.min, r=['df'], w=['df'])
                        P.cp('dve', dst[:, i:i + 1], df[:, k:k + 1], r=['df'], w=['dst%d' % k])
                        P.idma(idx_s[:, :], bass.IndirectOffsetOnAxis(ap=dst[:, i:i + 1], axis=0), tokid[:, i:i + 1], None,
                               NE * CAP - 1, r=['dst%d' % k, 'tokid'], w=['idx_s'])
                    P.tt('pool', Racc[:], Racc[:], Ms[:], ALU.add, r=['Racc', 'Ms'], w=['Racc'])
                P.barrier()

        if "F" in STAGES:
            with ExitStack() as st:
                sG = [sb("sG%d" % i, [128, 8, 512], F32, st) for i in range(2)]
                sU = [sb("sU%d" % i, [128, 8, 512], F32, st) for i in range(2)]
                sD = [sb("sD%d" % i, [128, 4, 1024], F32, st) for i in range(2)]
                bG = [sb("bG%d" % i, [128, 8, 512], BF16, st) for i in range(2)]
                bU = [sb("bU%d" % i, [128, 8, 512], BF16, st) for i in range(2)]
                bD = [sb("bD%d" % i, [128, 4, 1024], BF16, st) for i in range(2)]
                idt = [sb("idt%d" % i, [128, 1], I32, st) for i in range(2)]
                xe = [sb("xe%d" % i, [128, D], BF16, st) for i in range(2)]
                xeT = sb("xeT", [128, 8, 128], BF16, st)
                gsl = sb("gslF", [128, 512], F32, st)
                hid = sb("hidF", [128, 512], BF16, st)
                hidT = sb("hidT", [128, 4, 128], BF16, st)
                yr = sb("yrF", [128, D], F32, st)
                for e in range(NE):
                    p2 = e % 2
                    P.dma('sp', sG[p2][:], w_g[e].rearrange("(kc p) n -> p kc n", p=128), w=[('sG', p2)])
                    P.dma('sp', sU[p2][:], w_u[e].rearrange("(kc p) n -> p kc n", p=128), w=[('sU', p2)])
                    P.dma('sp', sD[p2][:], w_d[e].rearrange("(c p) n -> p c n", p=128), w=[('sD', p2)])
                    P.cp('dve', bG[p2][:], sG[p2][:], r=[('sG', p2)], w=[('bG', p2)])
                    P.cp('pool', bU[p2][:], sU[p2][:], r=[('sU', p2)], w=[('bU', p2)])
                    P.cp('act', bD[p2][:], sD[p2][:], r=[('sD', p2)], w=[('bD', p2)])
                    P.dma('sp', idt[p2][:], idx_s[e * CAP:(e + 1) * CAP, :], w=[('idt', p2)])
                    P.idma(xe[p2][:, :], None, hn_s[:, :], bass.IndirectOffsetOnAxis(ap=idt[p2][:, :], axis=0), TOK - 1,
                           r=[('idt', p2)], w=[('xe', p2)])
                    for c in range(8):
                        P.tr(psB[0][:, c * 128:(c + 1) * 128], xe[p2][:, c * 128:(c + 1) * 128], identb[:],
                             r=[('xe', p2), 'identb'], w=['psB0'])
                    P.cp('dve', xeT[:], psB[0][:, :].rearrange("p (a b) -> p a b", a=8), r=['psB0'], w=['xeT'])
                    for kc in range(8):
                        P.mm(psF[0][:, :], xeT[:, kc, :], bG[p2][:, kc, :], start=(kc == 0), stop=(kc == 7), r=['xeT', ('bG', p2)], w=['psF0'])
                    for kc in range(8):
                        P.mm(psF[1][:, :], xeT[:, kc, :], bU[p2][:, kc, :], start=(kc == 0), stop=(kc == 7), r=['xeT', ('bU', p2)], w=['psF1'])
                    P.act(gsl[:], psF[0][:, :], AF.Silu, r=['psF0'], w=['gsl'])
                    P.tt('dve', hid[:], gsl[:], psF[1][:, :], ALU.mult, r=['gsl', 'psF1'], w=['hid'])
                    for c in range(4):
                        P.tr(psB[1][:, c * 128:(c + 1) * 128], hid[:, c * 128:(c + 1) * 128], identb[:], r=['hid', 'identb'], w=['psB1'])
                    P.cp('act', hidT[:], psB[1][:, 0:512].rearrange("p (a b) -> p a b", a=4), r=['psB1'], w=['hidT'])
                    for cb in range(2):
                        kq = 'psF%d' % (2 + cb)
                        for c in range(4):
                            P.mm(psF[2 + cb][:, :], hidT[:, c, :], bD[p2][:, c, cb * 512:(cb + 1) * 512], start=(c == 0), stop=(c == 3),
                                 r=['hidT', ('bD', p2)], w=[kq])
                        P.cp('dve' if cb == 0 else 'act', yr[:, cb * 512:(cb + 1) * 512], psF[2 + cb][:, :], r=[kq], w=[('yr', cb)])
                    P.dma('sp', yrow_s[e * CAP:(e + 1) * CAP, :], yr[:], r=[('yr', 0), ('yr', 1)])
                P.barrier()

        if "G" in STAGES:
            with ExitStack() as st:
                xmt = [sb("xmtG%d" % i, [128, D], F32, st) for i in range(2)]
                ya = [sb("yaG%d" % i, [128, D], F32, st) for i in range(2)]
                yb = [sb("ybG%d" % i, [128, D], F32, st) for i in range(2)]
                o1 = [sb("o1G%d" % i, [128, D], F32, st) for i in range(2)]
                for i in range(16):
                    p2 = i % 2
                    ts_ = slice(i * 128, (i + 1) * 128)
                    P.dma('sp', xmt[p2][:], xmid_s[ts_, :], w=[('xmt', p2)])
                    P.memset('pool', ya[p2][:], 0.0, w=[('ya', p2)])
                    P.memset('pool', yb[p2][:], 0.0, w=[('yb', p2)])
                    P.idma(ya[p2][:, :], None, yrow_s[:, :], bass.IndirectOffsetOnAxis(ap=dstA[:, i:i + 1], axis=0), None,
                           r=['dst0'], w=[('ya', p2)])
                    P.idma(yb[p2][:, :], None, yrow_s[:, :], bass.IndirectOffsetOnAxis(ap=dstB[:, i:i + 1], axis=0), None,
                           r=['dst1'], w=[('yb', p2)])
                    P.stt('dve', o1[p2][:], ya[p2][:], gts[:, i, 0:1], xmt[p2][:], ALU.mult, ALU.add,
                          r=[('ya', p2), ('xmt', p2), 'gts'], w=[('o1', p2)])
                    P.stt('dve', o1[p2][:], yb[p2][:], gts[:, i, 1:2], o1[p2][:], ALU.mult, ALU.add,
                          r=[('yb', p2), ('o1', p2), 'gts'], w=[('o1', p2)])
                    P.dma('sp', out[ts_, :], o1[p2][:], r=[('o1', p2)])

        P.final_wait('sp')
        with ExitStack() as es:
            sems = {}
            for s in sorted(P.semnames):
                sems[s] = es.enter_context(nc.semaphore("s_" + s))
            block = es.enter_context(nc.Block())
            P.emit(block, sems)
    return nc


def _host_consts():
    i = np.arange(128)
    ident = np.eye(128, dtype=np.float32)
    U = (i[:, None] <= i[None, :]).astype(np.float32)
    SL = (i[:, None] > i[None, :]).astype(np.float32)
    ones = np.ones((128, 128), np.float32)
    return np.concatenate([ident, U, SL, ones], axis=1)


def _host_consts2():
    i = np.arange(128)
    su = (i[:, None] < i[None, :]).astype(np.float32)
    blk = ((i[:, None] // 64) == (i[None, :] // 64)).astype(np.float32)
    iot = np.tile((np.arange(64, dtype=np.float32) * CAP)[None, :], (128, 1))
    return np.ascontiguousarray(np.concatenate([su, blk, iot], axis=1))


def _t5_bucket(dist):
    nb, md = 32, 2048
    me = nb // 2
    large = me + (np.log(np.maximum(dist, me) / me) / np.log(md / me) * (nb - me)).astype(np.int32)
    return np.where(dist < me, dist, np.minimum(large, nb - 1)).astype(np.int32)


def prep_inputs(inp):
    f = np.float32
    x = np.asarray(inp['x'], f)
    cst = _host_consts()

    def chunked(v, n):
        return np.ascontiguousarray(np.asarray(v, f).reshape(n, 128).T)
    conv_w = np.asarray(inp['conv_w'], f)[0]
    cw = np.ascontiguousarray(conv_w.reshape(4, 20, 128).transpose(2, 0, 1))
    cb = chunked(np.asarray(inp['conv_b'])[0], 20)
    dsk = np.ascontiguousarray(np.repeat(np.asarray(inp['d_skip'], f)[0], 64).reshape(16, 128).T)
    qnw = np.tile(np.asarray(inp['q_norm_w'], f)[0], 2).reshape(128, 1)
    knw = np.tile(np.asarray(inp['k_norm_w'], f)[0], 2).reshape(128, 1)
    rb = np.asarray(inp['rel_bias'], f)
    kq = np.arange(128)
    biasT = np.full((128, 24, 2, 128), -30000.0, f)
    for g, r in enumerate((1, 4, 16)):
        for hh in range(8):
            h = g * 8 + hh
            offp = kq[None, :] + 128 - kq[:, None]
            offc = kq[None, :] - kq[:, None]
            tp = rb[_t5_bucket(np.clip(offp, 0, None) * r), h]
            tcur = rb[_t5_bucket(np.clip(offc, 0, None) * r), h]
            biasT[:, h, 0, :] = np.where((offp >= 0) & (offp <= 128), tp, -30000.0)
            biasT[:, h, 1, :] = np.where((offc >= 0) & (offc <= 128), tcur, -30000.0)
    w_rt = np.concatenate([np.asarray(inp['w_coarse'], f)[0], np.asarray(inp['w_fine'], f)[0]], axis=1)
    b_rt = np.concatenate([np.asarray(inp['b_coarse'], f)[0], np.asarray(inp['b_fine'], f)[0]])[None, :]
    shared = {
        'cst': cst, 'w_in': np.asarray(inp['w_in'], f)[0], 'nmw': chunked(np.asarray(inp['norm_mix_w'])[0], 8),
        'conv_w': cw, 'conv_b': cb, 'dtb': np.asarray(inp['dt_bias'], f), 'alog': np.asarray(inp['a_log'], f),
        'dsk': dsk, 'snw': np.asarray(inp['ssm_norm_w'], f), 'w_ssm': np.asarray(inp['w_ssm_proj'], f)[0],
        'qnw': qnw, 'knw': knw, 'biasT': np.ascontiguousarray(biasT.reshape(128, -1)),
        'w_attn': np.asarray(inp['w_attn_proj'], f)[0], 'w_out': np.asarray(inp['w_out'], f)[0],
        'nfw': chunked(np.asarray(inp['norm_ffn_w'])[0], 8), 'w_rt': np.ascontiguousarray(w_rt), 'b_rt': b_rt,
        'nfw_row': np.asarray(inp['norm_ffn_w'], f), 'cst2': _host_consts2(),
        'tokid_d': np.ascontiguousarray(np.arange(16, dtype=np.int32)[None, :] * 128 + np.arange(128, dtype=np.int32)[:, None]),
        'w_g': np.asarray(inp['w_gate_exp'], f)[0][:(NE if "F" in STAGES else 1)],
        'w_u': np.asarray(inp['w_up_exp'], f)[0][:(NE if "F" in STAGES else 1)],
        'w_d': np.asarray(inp['w_down_exp'], f)[0][:(NE if "F" in STAGES else 1)],
    }
    maps = []
    for c in range(8):
        b, q = c // 4, c % 4
        m = dict(shared)
        m['xm'] = np.ascontiguousarray(x[b, q * TOK:(q + 1) * TOK])
        xpv = np.zeros((PRE, D), f)
        if q > 0:
            xpv[PRE - q * TOK:] = x[b, 0:q * TOK]
        m['xp'] = xpv
        pmv = np.ones((128, NBLK), f)
        for j in range(12):
            pmv[:, j] = 1.0 if j * 512 >= PRE - q * TOK else 0.0
        m['pm'] = pmv
        m['hv'] = np.full((128, 1), 1.0 if q > 0 else 0.0, f)
        maps.append(m)
    return maps


def kernel(**inputs):
    maps = prep_inputs(inputs)
    nc = build_program()
    res = run_bass_kernel_spmd(nc, maps, core_ids=list(range(8)))
    outs = [np.asarray(r['out']) for r in res.results]
    full = np.stack([np.concatenate(outs[0:4], 0), np.concatenate(outs[4:8], 0)], 0)
    return full.astype(np.float32)
```

```python
import numpy as np
import concourse.bass as bass
import concourse.mybir as mybir
from concourse.bass_utils import run_bass_kernel_spmd

F32, BF16, I32 = mybir.dt.float32, mybir.dt.bfloat16, mybir.dt.int32
ALU = mybir.AluOpType
AF = mybir.ActivationFunctionType
AX = mybir.AxisListType

D = 1024
DI = 2048
NH = 32
TOK = 2048
PRE = 6144
NBLK = 16
EPS = 1e-6
C_Z, C_X, C_B, C_C, C_DT, C_Q, C_K, C_V, C_GS, C_GA = 0, 2048, 4096, 4352, 4608, 4640, 6176, 7712, 9248, 10272
NE = 64
CAP = 128
ENGS = ['pe', 'act', 'dve', 'pool', 'sp']
STAGES = "ABCDEFG"
DEBUG = None
LIM_BLK = None
LIM_PART = 9


class Prog:
    def __init__(self, nc):
        self.nc = nc
        self.ops = {e: [] for e in ENGS}
        self.cnt = {e: 0 for e in ENGS}
        self.seen = {e: {} for e in ENGS}
        self.lastw = {}
        self.readers = {}
        self.dcnt = {e: 0 for e in ENGS}
        self.NDS = 6
        self.pending = {e: {} for e in ENGS}
        self.semnames = set(ENGS)

    def _add(self, eng, fn, r, w, dma):
        waits = dict(self.pending[eng])
        self.pending[eng] = {}
        psr = [k for k in r if isinstance(k, str) and k.startswith('ps')]
        if psr:
            w = list(w) + psr

        def need(tok):
            if tok is None:
                return
            s, v = tok
            if s == eng and eng == 'pe':
                return
            if waits.get(s, 0) < v:
                waits[s] = v
        for k in r:
            need(self.lastw.get(k))
        for k in w:
            need(self.lastw.get(k))
            for s, v in self.readers.get(k, {}).items():
                need((s, v))
        if dma:
            n = self.dcnt[eng]
            self.dcnt[eng] += 1
            sname = "%s_d%d" % (eng, n % self.NDS)
            self.semnames.add(sname)
            val = 16 * (n // self.NDS + 1)
            if n >= self.NDS:
                need((sname, val - 16))
            tok = (sname, val)
        else:
            self.cnt[eng] += 1
            tok = (eng, self.cnt[eng])
        wl = []
        for s, v in waits.items():
            if self.seen[eng].get(s, 0) < v:
                self.seen[eng][s] = v
                wl.append((s, v))
        self.ops[eng].append((wl, fn, tok, dma))
        for k in w:
            self.lastw[k] = tok
            self.readers[k] = {}
        for k in r:
            d = self.readers.setdefault(k, {})
            if d.get(tok[0], 0) < tok[1]:
                d[tok[0]] = tok[1]
        return tok

    def barrier(self):
        toks = {}
        for e in ENGS:
            if self.cnt[e]:
                toks[e] = self.cnt[e]
            for i in range(min(self.dcnt[e], self.NDS)):
                n_last = ((self.dcnt[e] - 1 - i) // self.NDS) * self.NDS + i
                toks["%s_d%d" % (e, i)] = 16 * (n_last // self.NDS + 1)
        for e in ENGS:
            for s, v in toks.items():
                if self.pending[e].get(s, 0) < v:
                    self.pending[e][s] = v
        self.lastw = {}
        self.readers = {}

    def final_wait(self, eng):
        self.barrier()
        wl = [(s, v) for s, v in self.pending[eng].items() if self.seen[eng].get(s, 0) < v]
        self.ops[eng].append((wl, None, None, False))

    def mm(self, out, lhsT, rhs, start=True, stop=True, r=(), w=()):
        return self._add('pe', lambda h: h.matmul(out, lhsT, rhs, start=start, stop=stop, skip_group_check=True), r, w, False)

    def tr(self, out, in_, ident, r=(), w=()):
        return self._add('pe', lambda h: h.transpose(out, in_, ident), r, w, False)

    def act(self, out, in_, func, bias=0.0, scale=1.0, accum=None, r=(), w=()):
        if accum is None:
            return self._add('act', lambda h: h.activation(out, in_, func, bias=bias, scale=scale), r, w, False)
        return self._add('act', lambda h: h.activation(out, in_, func, bias=bias, scale=scale, accum_out=accum), r, w, False)

    def ts(self, eng, out, in0, s1, s2, op0, op1=None, r=(), w=()):
        if op1 is None:
            return self._add(eng, lambda h: h.tensor_scalar(out, in0, s1, None, op0), r, w, False)
        return self._add(eng, lambda h: h.tensor_scalar(out, in0, s1, s2, op0, op1), r, w, False)

    def tt(self, eng, out, in0, in1, op, r=(), w=()):
        return self._add(eng, lambda h: h.tensor_tensor(out, in0, in1, op), r, w, False)

    def stt(self, eng, out, in0, sc, in1, op0, op1, r=(), w=()):
        return self._add(eng, lambda h: h.scalar_tensor_tensor(out, in0, sc, in1, op0, op1), r, w, False)

    def cp(self, eng, out, in_, r=(), w=()):
        if eng == 'act':
            return self._add(eng, lambda h: h.copy(out, in_), r, w, False)
        return self._add(eng, lambda h: h.tensor_copy(out, in_), r, w, False)

    def red(self, eng, out, in_, op, r=(), w=()):
        return self._add(eng, lambda h: h.tensor_reduce(out, in_, AX.X, op), r, w, False)

    def memset(self, eng, out, val, r=(), w=()):
        return self._add(eng, lambda h: h.memset(out, val), r, w, False)

    def dma(self, eng, out, in_, r=(), w=()):
        return self._add(eng, lambda h: h.dma_start(out=out, in_=in_), r, w, True)

    def idma(self, out, out_off, in_, in_off, bc, r=(), w=()):
        return self._add('pool', lambda h: h.indirect_dma_start(
            out=out, out_offset=out_off, in_=in_, in_offset=in_off), r, w, True)

    def emit(self, block, sems):
        hmap = {'pe': 'tensor', 'act': 'scalar', 'dve': 'vector', 'pool': 'gpsimd', 'sp': 'sync'}
        for e in ENGS:
            ops = self.ops[e]
            if not ops:
                continue

            def body(h, ops=ops):
                for wl, fn, tok, dma in ops:
                    for s, v in wl:
                        h.wait_ge(sems[s], v)
                    if fn is not None:
                        fn(h).then_inc(sems[tok[0]], 16 if dma else 1)
            getattr(block, hmap[e])(body)


def bc_free(ap2d, n):
    a = list(ap2d.ap)
    return bass.AP(ap2d.tensor, ap2d.offset, [list(a[0]), list(a[1]), [0, n]])


def bc_mid(ap2d, n):
    a = list(ap2d.ap)
    return bass.AP(ap2d.tensor, ap2d.offset, [list(a[0]), [0, n], list(a[1])])


def build_program():
    nc = bass.Bass("TRN2", target_bir_lowering=False)
    P = Prog(nc)

    def din(name, shape, dt=F32):
        return nc.dram_tensor(name, list(shape), dt, kind="ExternalInput").ap()

    def dscr(name, shape, dt=F32):
        return nc.dram_tensor(name, list(shape), dt, kind="Internal").ap()

    xm = din("xm", [TOK, D])
    xp = din("xp", [PRE, D])
    pm = din("pm", [128, NBLK])
    hv = din("hv", [128, 1])
    cst = din("cst", [128, 4 * 128])
    w_in = din("w_in", [D, 11296])
    nmw = din("nmw", [128, 8])
    conv_w = din("conv_w", [128, 4, 20])
    conv_b = din("conv_b", [128, 20])
    dtb = din("dtb", [1, NH])
    alog = din("alog", [1, NH])
    dsk = din("dsk", [128, 16])
    snw = din("snw", [1, DI])
    w_ssm = din("w_ssm", [DI, D])
    qnw = din("qnw", [128, 1])
    knw = din("knw", [128, 1])
    biasT = din("biasT", [128, 24 * 2 * 128])
    w_attn = din("w_attn", [512, D])
    w_out = din("w_out", [D, D])
    nfw = din("nfw", [128, 8])
    w_rt = din("w_rt", [D, 72])
    b_rt = din("b_rt", [1, 72])
    nfw_row = din("nfw_row", [1, D])
    cst2 = din("cst2", [128, 320])
    tokid_d = din("tokid_d", [128, 16], I32)
    nE = NE if "F" in STAGES else 1
    w_g = din("w_g", [nE, D, 512])
    w_u = din("w_u", [nE, D, 512])
    w_d = din("w_d", [nE, 512, D])
    out = nc.dram_tensor("out", [TOK, D], F32, kind="ExternalOutput").ap()
    dbg = None
    if DEBUG is not None:
        dbg = nc.dram_tensor("dbg", list(DEBUG), F32, kind="ExternalOutput").ap()

    y_s = dscr("y_s", [TOK, DI])
    hT_s = dscr("hT_s", [2, 128, 8 * TOK], BF16)
    mg_s = dscr("mg_s", [TOK, D])
    xmid_s = dscr("xmid_s", [TOK, D])
    hn_s = dscr("hn_s", [TOK, D], BF16)
    idx_s = dscr("idx_s", [NE * CAP + 128, 1], I32)
    yrow_s = dscr("yrow_s", [NE * CAP + 128, D])

    w_in_v = w_in.rearrange("(kc p) n -> p kc n", p=128)

    from contextlib import ExitStack
    with ExitStack() as top:
        def sb(name, shape, dt=F32, stack=top):
            return stack.enter_context(nc.sbuf_tensor(name, list(shape), dt))

        def ps(name, shape, dt=F32, stack=top):
            return stack.enter_context(nc.psum_tensor(name, list(shape), dt))

        psF = [ps("psF%d" % i, [128, 512]) for i in range(6)]
        psB = [ps("psB%d" % i, [128, 1024], BF16) for i in range(2)]
        cstt = sb("cstt", [128, 512])
        identf, Umat, SLmat, onesf = cstt[:, 0:128], cstt[:, 128:256], cstt[:, 256:384], cstt[:, 384:512]
        identb = sb("identb", [128, 128], BF16)
        cstb = sb("cstb", [128, 384], BF16)
        Ub, SLb, onesb = cstb[:, 0:128], cstb[:, 128:256], cstb[:, 256:384]
        P.dma('sp', cstt[:], cst[:, :], w=['cst'])
        P.cp('dve', identb[:], identf, r=['cst'], w=['identb'])
        P.cp('dve', cstb[:], cstt[:, 128:512], r=['cst'], w=['cst'])

        if "A" in STAGES:
            with ExitStack() as st:
                Wx = sb("Wx", [128, 8, 2560], BF16, st)
                Wdt = sb("Wdt", [128, 8, 32], BF16, st)
                Rst = sb("Rst", [128, 2 * NH * 128], BF16, st)
                Rf = Rst[:].bitcast(F32)
                Rh = Rst[:, 0:NH * 128].rearrange("p (a b) -> p a b", a=NH)
                Rl = Rst[:, NH * 128:2 * NH * 128].rearrange("p (a b) -> p a b", a=NH)
                for kc in range(8):
                    P.dma('sp', Rf[:, 0:2048], w_in_v[:, kc, C_X:C_X + 2048], w=[('wst', 0)])
                    P.cp('pool', Wx[:, kc, 0:2048], Rf[:, 0:2048], r=[('wst', 0)], w=[('Wx', kc)])
                    P.dma('sp', Rf[:, 2048:2560], w_in_v[:, kc, C_X + 2048:C_X + 2560], w=[('wst', 1)])
                    P.cp('dve', Wx[:, kc, 2048:2560], Rf[:, 2048:2560], r=[('wst', 1)], w=[('Wx', kc)])
                P.dma('sp', Rf[:, 2560:2816].rearrange("p (a b) -> p a b", a=8), w_in_v[:, :, C_DT:C_DT + 32], w=[('wst', 2)])
                P.cp('dve', Wdt[:], Rf[:, 2560:2816].rearrange("p (a b) -> p a b", a=8), r=[('wst', 2)], w=['Wdt'])
                nmw_t = sb("nmw_t", [128, 8], F32, st)
                cw_t = sb("cw_t", [128, 4, 20], F32, st)
                cb_t = sb("cb_t", [128, 20], F32, st)
                dsk_t = sb("dsk_t", [128, 16], F32, st)
                pm_t = sb("pm_t", [128, NBLK], F32, st)
                dtb_t = sb("dtb_t", [128, NH], F32, st)
                a_t = sb("a_t", [128, NH], F32, st)
                P.dma('sp', nmw_t[:], nmw[:, :], w=['nmw'])
                P.dma('sp', cw_t[:], conv_w[:, :, :], w=['cw'])
                P.dma('sp', cb_t[:], conv_b[:, :], w=['cb'])
                P.dma('sp', dsk_t[:], dsk[:, :], w=['dsk'])
                P.dma('sp', pm_t[:], pm[:, :], w=['pm'])
                P.dma('sp', dtb_t[:], dtb[0].partition_broadcast(128), w=['dtb'])
                P.dma('sp', a_t[:], alog[0].partition_broadcast(128), w=['a'])
                P.act(a_t[:], a_t[:], AF.Exp, r=['a'], w=['a'])
                P.ts('dve', a_t[:], a_t[:], -1.0, None, ALU.mult, r=['a'], w=['a'])
                convdiag = sb("convdiag", [128, 4, 20, 128], BF16, st)
                for j in range(4):
                    for ct in range(20):
                        P.ts('dve', convdiag[:, j, ct, :], identf, cw_t[:, j, ct:ct + 1], None, ALU.mult,
                             r=['cst', 'cw'], w=[('cd', j, ct)])
                dskdiag = sb("dskdiag", [128, 16, 128], BF16, st)
                for ct in range(16):
                    P.ts('dve', dskdiag[:, ct, :], identf, dsk_t[:, ct:ct + 1], None, ALU.mult,
                         r=['cst', 'dsk'], w=[('dd', ct)])

                xt = [sb("xt%d" % i, [128, D], F32, st) for i in range(2)]
                xsq = sb("xsq", [128, D], F32, st)
                xn = sb("xn", [128, D], BF16, st)
                ss = sb("ss", [128, 2], F32, st)
                hT = sb("hT", [128, 8, 512], BF16, st)
                xin = sb("xin", [128, 20, 515], BF16, st)
                xbcT = sb("xbcT", [128, 20, 512], BF16, st)
                sm = sb("sm", [128, 14, NH], F32, st)
                smb = sb("smb", [128, 2, NH], BF16, st)
                expD = sb("expD", [128, NH, 128], BF16, st)
                MT = sb("MT", [128, NH, 128], BF16, st)
                cbm = sb("cbm", [128, 2, 128], BF16, st)
                xdt = sb("xdt", [128, DI], BF16, st)
                xw = sb("xw", [128, DI], BF16, st)
                Btok = sb("Btok", [128, 256], BF16, st)
                S = sb("S", [128, 2, 1024], F32, st)
                Sb = sb("Sb", [128, 2, 1024], BF16, st)
                ydg = sb("ydg", [128, DI], F32, st)
                yo = sb("yo", [128, DI], F32, st)
                P.memset('dve', S[:], 0.0, w=['S'])
                P.memset('dve', xin[:], 0.0, w=[('xin', c) for c in range(20)])
                tcount = 0
                for b in range(NBLK):
                    if LIM_BLK is not None and b not in LIM_BLK:
                        continue
                    main = b >= 12
                    src = xm if main else xp
                    row0 = (b - 12) * 512 if main else b * 512
                    nct = 20 if b >= 11 else 18
                    for t in range(4):
                        xb = xt[tcount % 2]
                        kx = ('xt', tcount % 2)
                        tcount += 1
                        P.dma('sp', xb[:], src[row0 + t * 128: row0 + (t + 1) * 128, :], w=[kx])
                        P.tt('pool', xsq[:], xb[:], xb[:], ALU.mult, r=[kx], w=['xsq'])
                        P.red('dve', ss[:, 0:1], xsq[:], ALU.add, r=['xsq'], w=['ss'])
                        P.ts('dve', ss[:, 1:2], ss[:, 0:1], 1.0 / D, EPS, ALU.mult, ALU.add, r=['ss'], w=['ss1'])
                        P.act(ss[:, 1:2], ss[:, 1:2], AF.Sqrt, r=['ss1'], w=['ss1'])
                        P._add('dve', lambda h, a=ss: h.reciprocal(a[:, 1:2], a[:, 1:2]), ['ss1'], ['ss1'], False)
                        P._add('act', lambda h, o=xn, i=xb, a=ss: h.mul(o[:], i[:], a[:, 1:2]), [kx, 'ss1'], ['xn'], False)
                        for kc in range(8):
                            P.tr(psB[0][:, kc * 128:(kc + 1) * 128], xn[:, kc * 128:(kc + 1) * 128], identb[:],
                                 r=['xn', 'identb'], w=['psB0'])
                        P.tt('dve', hT[:, :, t * 128:(t + 1) * 128], psB[0][:, :].rearrange("p (a b) -> p a b", a=8),
                             bc_free(nmw_t[:, :], 128), ALU.mult, r=['psB0', 'nmw'], w=[('hT', t)])
                    hkeys = [('hT', t) for t in range(4)]
                    if b >= 8:
                        seg = 1 if main else 0
                        c0 = ((b - 8) % 4) * 512
                        for kc in range(8):
                            P.dma('sp', hT_s[seg, :, kc * TOK + c0: kc * TOK + c0 + 512], hT[:, kc, :], r=hkeys)
                    if LIM_PART < 1:
                        continue
                    if b > 0:
                        P.cp('pool', xin[:, :, 0:3], xin[:, :, 512:515], r=[('xin', c) for c in range(20)],
                             w=[('xin', c) for c in range(20)])
                    for ct in range(nct):
                        pa, pb = psF[2 + 2 * (ct % 2)], psF[3 + 2 * (ct % 2)]
                        ka, kb = 'psF%d' % (2 + 2 * (ct % 2)), 'psF%d' % (3 + 2 * (ct % 2))
                        for kc in range(8):
                            P.mm(pa[:, :], Wx[:, kc, ct * 128:(ct + 1) * 128], hT[:, kc, :], start=(kc == 0), stop=(kc == 7),
                                 r=[('Wx', kc)] + hkeys, w=[ka])
                        P.cp('act', xin[:, ct, 3:515], pa[:, :], r=[ka], w=[('xin', ct)])
                        for j in range(4):
                            P.mm(pb[:, :], convdiag[:, j, ct, :], xin[:, ct, j:j + 512], start=(j == 0), stop=(j == 3),
                                 r=[('cd', j, ct), ('xin', ct)], w=[kb])
                        P.act(xbcT[:, ct, :], pb[:, :], AF.Silu, bias=cb_t[:, ct:ct + 1], r=[kb, 'cb'], w=[('xbc', ct)])
                    if LIM_PART == 1 and DEBUG is not None:
                        P.dma('sp', dbg[0:128, 0:2], ss[:, :], r=['ss', 'ss1'])
                        P.dma('sp', dbg[0:128, 8:16], nmw_t[:, :], r=['nmw'])
                        P.cp('dve', yo[:, 0:512], hT[:, 0, :], r=hkeys, w=['yo'])
                        P.cp('dve', yo[:, 512:1024], Wx[:, 0, 0:512], r=[('Wx', 0)], w=['yo'])
                        P.cp('dve', yo[:, 1024:1536], xin[:, 0, 3:515], r=[('xin', 0)], w=['yo'])
                        P.cp('dve', yo[:, 1536:1664], convdiag[:, 0, 0, :], r=[('cd', 0, 0)], w=['yo'])
                        P.cp('dve', yo[:, 1664:1792], identb[:], r=['identb'], w=['yo'])
                        P.cp('dve', yo[:, 1792:2048], xn[:, 0:256], r=['xn'], w=['yo'])
                        P.dma('sp', dbg[0:128, 16:16 + 2032], yo[:, 0:2032], r=['yo'])
                        for ct in range(16):
                            P.cp('dve', ydg[:, 0:512], xbcT[:, ct, :], r=[('xbc', ct)], w=['ydg'])
                            P.dma('sp', dbg[128 + ct * 128:128 + (ct + 1) * 128, 0:512], ydg[:, 0:512], r=['ydg'])
                    for t in range(4):
                        if LIM_PART < 2:
                            continue
                        cs_ = slice(t * 128, (t + 1) * 128)
                        dtr, av, ex, ln, dtv, adt, cs, cst_, dfs, dte, dtot, dtw = [sm[:, i, :] for i in range(12)]
                        for kc in range(8):
                            P.mm(psF[0][:, 0:32], hT[:, kc, cs_], Wdt[:, kc, :], start=(kc == 0), stop=(kc == 7),
                                 r=[('hT', t), 'Wdt'], w=['psF0'])
                        P.tt('dve', dtr, psF[0][:, 0:32], dtb_t[:], ALU.add, r=['psF0', 'dtb'], w=['dtr'])
                        P.act(ex, dtr, AF.Exp, r=['dtr'], w=['ex'])
                        P.act(dtv, ex, AF.Ln, bias=1.0, r=['ex'], w=['dtv'])
                        if not main:
                            P.ts('dve', dtv, dtv, pm_t[:, b:b + 1], None, ALU.mult, r=['dtv', 'pm'], w=['dtv'])
                        P.tt('dve', adt, dtv, a_t[:], ALU.mult, r=['dtv', 'a'], w=['adt'])
                        ahi, alo = smb[:, 0, :], smb[:, 1, :]
                        ahf, alf = sm[:, 12, :], sm[:, 13, :]
                        P.cp('dve', ahi, adt, r=['adt'], w=['ahi'])
                        P.cp('dve', ahf, ahi, r=['ahi'], w=['ahf'])
                        P.tt('dve', alf, adt, ahf, ALU.subtract, r=['adt', 'ahf'], w=['alf'])
                        P.cp('dve', alo, alf, r=['alf'], w=['alo'])
                        P.mm(psF[0][:, 32:64], Ub, ahi, start=True, stop=False, r=['cst', 'ahi'], w=['psF0'])
                        P.mm(psF[0][:, 32:64], Ub, alo, start=False, stop=True, r=['cst', 'alo'], w=['psF0'])
                        P.mm(psF[0][:, 64:96], onesb, ahi, start=True, stop=False, r=['cst', 'ahi'], w=['psF0'])
                        P.mm(psF[0][:, 64:96], onesb, alo, start=False, stop=True, r=['cst', 'alo'], w=['psF0'])
                        P.cp('dve', cs, psF[0][:, 32:64], r=['psF0'], w=['cs'])
                        P.act(dfs, psF[0][:, 32:64], AF.Exp, r=['psF0'], w=['dfs'])
                        P.act(dtot, psF[0][:, 64:96], AF.Exp, r=['psF0'], w=['dtot'])
                        P.tt('dve', dte, psF[0][:, 64:96], cs, ALU.subtract, r=['psF0', 'cs'], w=['dte'])
                        P.act(dte, dte, AF.Exp, r=['dte'], w=['dte'])
                        P.tt('dve', dtw, dte, dtv, ALU.mult, r=['dte', 'dtv'], w=['dtw'])
                        for half in range(2):
                            for c8 in range(8):
                                ct = half * 8 + c8
                                P.tr(psB[0][:, c8 * 128:(c8 + 1) * 128], xbcT[:, ct, cs_], identb[:],
                                     r=[('xbc', ct), 'identb'], w=['psB0'])
                            hs = slice(half * 1024, (half + 1) * 1024)
                            pv = psB[0][:, :].rearrange("p (h d) -> p h d", h=16)
                            hh = slice(half * 16, (half + 1) * 16)
                            P.tt('dve', xw[:, hs].rearrange("p (h d) -> p h d", h=16), pv, bc_free(dtw[:, hh], 64), ALU.mult,
                                 r=['psB0', 'dtw'], w=[('xw', half)])
                            if main:
                                P.tt('dve', xdt[:, hs].rearrange("p (h d) -> p h d", h=16), pv, bc_free(dtv[:, hh], 64),
                                     ALU.mult, r=['psB0', 'dtv'], w=[('xdt', half)])
                        for g in range(2):
                            P.tr(psB[1][:, g * 128:(g + 1) * 128], xbcT[:, 16 + g, cs_], identb[:],
                                 r=[('xbc', 16 + g), 'identb'], w=['psB1'])
                        P.cp('dve', Btok[:], psB[1][:, 0:256], r=['psB1'], w=['Btok'])
                        if main:
                            P.tt('pool', Rh, bc_mid(Ub, NH), bc_free(ahi, 128), ALU.mult, r=['cst', 'ahi'], w=['Rm', ('wst', 0), ('wst', 1), ('wst', 2)])
                            P.tt('pool', Rl, bc_mid(Ub, NH), bc_free(alo, 128), ALU.mult, r=['cst', 'alo'], w=['Rm', ('wst', 0), ('wst', 1), ('wst', 2)])
                            for g in range(2):
                                P.mm(psF[0][:, 128 + g * 128: 256 + g * 128], xbcT[:, 16 + g, cs_], xbcT[:, 18 + g, cs_],
                                     r=[('xbc', 16 + g), ('xbc', 18 + g)], w=['psF0'])
                            P.tt('dve', cbm[:], psF[0][:, 128:384].rearrange("p (g l) -> p g l", g=2), bc_mid(Umat, 2),
                                 ALU.mult, r=['psF0', 'cst'], w=['cbm'])
                            for h4 in range(8):
                                P.mm(psF[1][:, :], SLb, Rh[:, h4 * 4:(h4 + 1) * 4, :], start=True, stop=False, r=['cst', 'Rm'], w=['psF1'])
                                P.mm(psF[1][:, :], SLb, Rl[:, h4 * 4:(h4 + 1) * 4, :], start=False, stop=True, r=['cst', 'Rm'], w=['psF1'])
                                P.act(expD[:, h4 * 4:(h4 + 1) * 4, :], psF[1][:, :].rearrange("p (h l) -> p h l", h=4), AF.Exp,
                                      r=['psF1'], w=[('expD', h4)])
                            for g in range(2):
                                P.tt('dve', MT[:, g * 16:(g + 1) * 16, :], expD[:, g * 16:(g + 1) * 16, :],
                                     bc_mid(cbm[:, g, :], 16), ALU.mult,
                                     r=[('expD', h4) for h4 in range(g * 4, g * 4 + 4)] + ['cbm'], w=[('MT', g)])
                            P.cp('act', Sb[:], S[:], r=['S'], w=['Sb'])
                            for g in range(2):
                                for j in range(2):
                                    kq = 'psF%d' % (4 + j)
                                    P.mm(psF[4 + j][:, :], xbcT[:, 18 + g, cs_], Sb[:, g, j * 512:(j + 1) * 512],
                                         r=[('xbc', 18 + g), 'Sb'], w=[kq])
                                    P.cp('act', yo[:, g * 1024 + j * 512: g * 1024 + (j + 1) * 512], psF[4 + j][:, :],
                                         r=[kq], w=[('yo', g, j)])
                                for j in range(2):
                                    kq = 'psF%d' % (2 + j)
                                    for c4 in range(4):
                                        ct = g * 8 + j * 4 + c4
                                        P.mm(psF[2 + j][:, c4 * 128:(c4 + 1) * 128], xbcT[:, ct, cs_], dskdiag[:, ct, :],
                                             start=True, stop=False, r=[('xbc', ct), ('dd', ct)], w=[kq])
                                        for hh2 in range(2):
                                            h = ct * 2 + hh2
                                            P.mm(psF[2 + j][:, c4 * 128 + hh2 * 64: c4 * 128 + (hh2 + 1) * 64], MT[:, h, :],
                                                 xdt[:, h * 64:(h + 1) * 64], start=False, stop=True,
                                                 r=[('MT', g), ('xdt', h // 16)], w=[kq])
                                    for h8 in range(8):
                                        h = g * 16 + j * 8 + h8
                                        P.stt('dve', ydg[:, h * 64:(h + 1) * 64], yo[:, h * 64:(h + 1) * 64], dfs[:, h:h + 1],
                                              psF[2 + j][:, h8 * 64:(h8 + 1) * 64], ALU.mult, ALU.add,
                                              r=[('yo', g, j), 'dfs', kq], w=['ydg'])
                            tok0 = (b - 12) * 512 + t * 128
                            P.dma('sp', y_s[tok0:tok0 + 128, :], ydg[:], r=['ydg'])
                            if DEBUG is not None and STAGES == "A":
                                P.dma('sp', dbg[128 + tok0:128 + tok0 + 128, :], ydg[:], r=['ydg'])
                        for g in range(2):
                            P.tt('pool', S[:, g, :].rearrange("p (h d) -> p h d", h=16), S[:, g, :].rearrange("p (h d) -> p h d", h=16),
                                 bc_free(dtot[:, g * 16:(g + 1) * 16], 64), ALU.mult, r=['S', 'dtot', 'Sb'], w=['S'])
                            for j in range(2):
                                kq = 'psF%d' % (4 + j)
                                P.mm(psF[4 + j][:, :], Btok[:, g * 128:(g + 1) * 128], xw[:, g * 1024 + j * 512: g * 1024 + (j + 1) * 512],
                                     r=['Btok', ('xw', g)], w=[kq])
                                P.tt('dve', S[:, g, j * 512:(j + 1) * 512], S[:, g, j * 512:(j + 1) * 512], psF[4 + j][:, :], ALU.add,
                                     r=['S', kq], w=['S'])
                if DEBUG is not None and STAGES == "A" and LIM_PART > 1:
                    P.dma('sp', dbg[0:128, 0:2048], S[:].rearrange("p g c -> p (g c)"), r=['S'])
                P.barrier()

        def wload(dst, srcfn, nk, ncols, stg, name):
            for kc in range(nk):
                sgt = stg[kc % 2]
                P.dma('sp', sgt[:, 0:ncols], srcfn(kc), w=[('stg', sgt.name if hasattr(sgt, 'name') else id(sgt))])
                P.cp('dve' if kc % 2 == 0 else 'pool', dst[:, kc, :], sgt[:, 0:ncols],
                     r=[('stg', sgt.name if hasattr(sgt, 'name') else id(sgt))], w=[(name, kc)])

        def strided(t, part, s0, r, n):
            base = t[part, s0:s0 + 1]
            a = list(base.ap)
            return bass.AP(base.tensor, base.offset, [list(a[0]), [r, n]])

        def rstd_ops(dst, src_sum, scale, keyr, keyw):
            P.ts('dve', dst, src_sum, scale, EPS, ALU.mult, ALU.add, r=keyr, w=keyw)
            P.act(dst, dst, AF.Sqrt, r=keyw, w=keyw)
            P._add('dve', lambda h, a=dst: h.reciprocal(a, a), keyw, keyw, False)

        aoT = sb("aoT", [128, 4, TOK], BF16)
        ohA = sb("ohA", [128, 16, 64])
        ohB = sb("ohB", [128, 16, 64])
        gts = sb("gts", [128, 16, 2])
        dstA = sb("dstA", [128, 16], I32)
        dstB = sb("dstB", [128, 16], I32)
        cst2t = sb("cst2t", [128, 320])
        SUmat, blk1, iotae = cst2t[:, 0:128], cst2t[:, 128:256], cst2t[:, 256:320]
        tokid = sb("tokid", [128, 16], I32)
        P.dma('sp', cst2t[:], cst2[:, :], w=['cst2'])
        P.dma('sp', tokid[:], tokid_d[:, :], w=['tokid'])
        w_ssm_v = w_ssm.rearrange("(c p) n -> p c n", p=128)
        w_attn_v = w_attn.rearrange("(c p) n -> p c n", p=128)
        w_out_v = w_out.rearrange("(c p) n -> p c n", p=128)
        w_rt_v = w_rt.rearrange("(c p) n -> p c n", p=128)

        if "B" in STAGES:
            with ExitStack() as st:
                stg = [sb("stgB%d" % i, [128, 2048], F32, st) for i in range(2)]
                Wz = sb("Wz", [128, 8, 2048], BF16, st)
                Wss = sb("Wss", [128, 16, 1024], BF16, st)
                Wgs = sb("Wgs", [128, 8, 1024], BF16, st)
                wload(Wz, lambda kc: w_in_v[:, kc, C_Z:C_Z + 2048], 8, 2048, stg, 'Wz')
                wload(Wss, lambda c: w_ssm_v[:, c, :], 16, 1024, stg, 'Wss')
                wload(Wgs, lambda kc: w_in_v[:, kc, C_GS:C_GS + 1024], 8, 1024, stg, 'Wgs')
                snw_t = sb("snw_t", [128, DI], F32, st)
                P.dma('sp', snw_t[:], snw[0].partition_broadcast(128), w=['snw'])
                hTm = sb("hTm", [128, 8, TOK], BF16, st)
                for kc in range(8):
                    P.dma('sp', hTm[:, kc, :], hT_s[1, :, kc * TOK:(kc + 1) * TOK], w=[('hTm', kc)])
                hk = [('hTm', kc) for kc in range(8)]
                yt = sb("ytB", [128, DI], F32, st)
                zs = sb("zsB", [128, DI], F32, st)
                y5 = yt
                sq = zs
                y6 = sb("y6B", [128, DI], BF16, st)
                yT = sb("yTB", [128, 16, 128], BF16, st)
                sg = sb("sgB", [128, D], F32, st)
                mo = sb("moB", [128, D], F32, st)
                ssb = sb("ssB", [128, 4], F32, st)
                for i in range(16):
                    ts_ = slice(i * 128, (i + 1) * 128)
                    P.dma('sp', yt[:], y_s[ts_, :], w=['yt'])
                    for cb in range(4):
                        kq = 'psF%d' % cb
                        for kc in range(8):
                            P.mm(psF[cb][:, :], hTm[:, kc, ts_], Wz[:, kc, cb * 512:(cb + 1) * 512], start=(kc == 0), stop=(kc == 7),
                                 r=[('hTm', kc), ('Wz', kc)], w=[kq])
                        P.act(zs[:, cb * 512:(cb + 1) * 512], psF[cb][:, :], AF.Silu, r=[kq], w=[('zs', cb)])
                    zk = [('zs', cb) for cb in range(4)]
                    P.tt('dve', y5[:], yt[:], zs[:], ALU.mult, r=['yt'] + zk, w=['y5', 'yt'])
                    P.tt('pool', sq[:], y5[:], y5[:], ALU.mult, r=['y5', 'yt'], w=['sq'] + zk)
                    P.red('dve', ssb[:, 0:2], sq[:].rearrange("p (g c) -> p g c", g=2), ALU.add, r=['sq'] + zk, w=['ssb0'])
                    rstd_ops(ssb[:, 2:4], ssb[:, 0:2], 1.0 / 1024, ['ssb0'], ['ssb1'])
                    for g in range(2):
                        gs_ = slice(g * 1024, (g + 1) * 1024)
                        P.stt('dve', y6[:, gs_], y5[:, gs_], ssb[:, 2 + g:3 + g], snw_t[:, gs_], ALU.mult, ALU.mult,
                              r=['y5', 'yt', 'ssb1', 'snw'], w=[('y6', g)])
                    for half in range(2):
                        for c8 in range(8):
                            c = half * 8 + c8
                            P.tr(psB[half][:, c8 * 128:(c8 + 1) * 128], y6[:, c * 128:(c + 1) * 128], identb[:],
                                 r=[('y6', c // 8), 'identb'], w=['psB%d' % half])
                        P.cp('dve' if half == 0 else 'act', yT[:, half * 8:(half + 1) * 8, :],
                             psB[half][:, :].rearrange("p (a b) -> p a b", a=8), r=['psB%d' % half], w=[('yT', half)])
                    for cb in range(2):
                        kq = 'psF%d' % (4 + cb)
                        for c in range(16):
                            P.mm(psF[4 + cb][:, :], yT[:, c, :], Wss[:, c, cb * 512:(cb + 1) * 512], start=(c == 0), stop=(c == 15),
                                 r=[('yT', c // 8), ('Wss', c)], w=[kq])
                    for cb in range(2):
                        kq = 'psF%d' % cb
                        for kc in range(8):
                            P.mm(psF[cb][:, :], hTm[:, kc, ts_], Wgs[:, kc, cb * 512:(cb + 1) * 512], start=(kc == 0), stop=(kc == 7),
                                 r=[('hTm', kc), ('Wgs', kc)], w=[kq])
                        P.act(sg[:, cb * 512:(cb + 1) * 512], psF[cb][:, :], AF.Sigmoid, r=[kq], w=[('sg', cb)])
                        P.tt('dve', mo[:, cb * 512:(cb + 1) * 512], sg[:, cb * 512:(cb + 1) * 512], psF[4 + cb][:, :], ALU.mult,
                             r=[('sg', cb), 'psF%d' % (4 + cb)], w=[('mo', cb)])
                    P.dma('sp', mg_s[ts_, :], mo[:], r=[('mo', 0), ('mo', 1)])
                    if DEBUG is not None and STAGES[-1] == "B":
                        P.dma('sp', dbg[ts_, 0:1024], mo[:], r=[('mo', 0), ('mo', 1)])
                P.barrier()

        if "C" in STAGES:
            with ExitStack() as st:
                hTa = sb("hTa", [128, 8, 2 * TOK], BF16, st)
                for kc in range(8):
                    P.dma('sp', hTa[:, kc, 0:TOK], hT_s[0, :, kc * TOK:(kc + 1) * TOK], w=[('hTa', kc)])
                    P.dma('sp', hTa[:, kc, TOK:2 * TOK], hT_s[1, :, kc * TOK:(kc + 1) * TOK], w=[('hTa', kc)])
                stg = [sb("stgC%d" % i, [128, 1024], F32, st) for i in range(2)]
                biasb = sb("biasb", [128, 6, 1024], BF16, st)
                wload(biasb, lambda c: biasT[:, c * 1024:(c + 1) * 1024], 6, 1024, stg, 'biasb')
                biasv = biasb[:].rearrange("p c (a q) -> p (c a) q", q=128)
                hv_t = sb("hv_t", [128, 1], F32, st)
                qn_t = sb("qn_t", [128, 1], F32, st)
                kn_t = sb("kn_t", [128, 1], F32, st)
                P.dma('sp', hv_t[:], hv[:, :], w=['hv'])
                P.dma('sp', qn_t[:], qnw[:, :], w=['qn'])
                P.dma('sp', kn_t[:], knw[:, :], w=['kn'])
                P.ts('dve', qn_t[:], qn_t[:], 0.125, None, ALU.mult, r=['qn'], w=['qn'])
                knAB = sb("knAB", [128, 2], F32, st)
                P.memset('dve', knAB[:], 0.0, w=['knAB'])
                P.cp('dve', knAB[0:64, 0:1], kn_t[0:64, 0:1], r=['kn'], w=['knAB'])
                P.cp('dve', knAB[64:128, 1:2], kn_t[64:128, 0:1], r=['kn'], w=['knAB'])
                KTB = sb("KTB", [128, 2 * TOK], BF16, st)
                Emat = sb("Emat", [128, 4, 128], BF16, st)
                P.memset('dve', Emat[:], 0.0, w=['E'])
                P.memset('dve', Emat[:, 0, 0:64], 1.0, w=['E'])
                P.memset('dve', Emat[:, 1, 64:128], 1.0, w=['E'])
                P.ts('dve', Emat[:, 2:4, :], Emat[:, 0:2, :], hv_t[:, 0:1], None, ALU.mult, r=['E', 'hv'], w=['E'])
                Wq = sb("WqC", [128, 8, 128], BF16, st)
                Wk = sb("WkC", [128, 8, 128], BF16, st)
                Wv = sb("WvC", [128, 8, 128], BF16, st)
                KT = sb("KT", [128, 2 * TOK], BF16, st)
                QT = sb("QT", [128, TOK], BF16, st)
                qs2 = [sb("qsC%d" % i, [128, 512], F32, st) for i in range(2)]
                sq2 = [sb("sqC%d" % i, [128, 512], BF16, st) for i in range(2)]
                rs2 = [sb("rsC%d" % i, [128, 512], F32, st) for i in range(2)]
                blkb = sb("blkb", [128, 128], BF16, st)
                P.cp('dve', blkb[:], blk1, r=['cst2'], w=['blkb'])
                pvb = [psB[i][:].bitcast(F32) for i in range(2)]
                pcnt = [0, 0, 0]
                VA = sb("VA", [128, 32, 128], BF16, st)
                VB = sb("VB", [128, 32, 128], BF16, st)
                P.memset('pool', VA[:], 0.0, w=[('VA', t_) for t_ in range(32)])
                P.memset('pool', VB[:], 0.0, w=[('VB', t_) for t_ in range(32)])
                PT2 = [sb("PT%d" % i, [128, 512], BF16, st) for i in range(2)]
                acc = sb("accC", [128, 2, TOK], F32, st)
                hka = [('hTa', kc) for kc in range(8)]

                def proj_norm(Wt, wname, tok0, w_ap, dst, keyw, dst2=None, w_ap2=None):
                    pp = pcnt[0] % 2
                    pcnt[0] += 1
                    qs, sq, rs = qs2[pp], sq2[pp], rs2[pp]
                    ka, kb_ = 'psF%d' % pp, 'psF%d' % (2 + pp)
                    for kc in range(8):
                        P.mm(psF[pp][:, :], Wt[:, kc, :], hTa[:, kc, tok0:tok0 + 512], start=(kc == 0), stop=(kc == 7),
                             r=[(wname, kc), ('hTa', kc)], w=[ka])
                    P.cp('act', qs[:], psF[pp][:, :], r=[ka], w=[('qs', pp)])
                    P.tt('pool', sq[:], qs[:], qs[:], ALU.mult, r=[('qs', pp)], w=[('sq', pp)])
                    P.mm(psF[2 + pp][:, :], blkb[:], sq[:], r=['blkb', ('sq', pp)], w=[kb_])
                    rstd_ops(rs[:], psF[2 + pp][:, :], 1.0 / 64, [kb_], [('rs', pp)])
                    P.stt('dve', dst, qs[:], w_ap, rs[:], ALU.mult, ALU.mult, r=[('qs', pp), ('rs', pp), 'qn', 'kn', 'knAB'], w=[keyw])
                    if dst2 is not None:
                        P.stt('dve', dst2, qs[:], w_ap2, rs[:], ALU.mult, ALU.mult, r=[('qs', pp), ('rs', pp), 'knAB'], w=[keyw])

                for hp in range(4):
                    for g, r_ in enumerate((1, 4, 16)):
                        hcol = (g * 8 + hp * 2) * 64
                        for Wt, wn, c0 in ((Wq, 'Wq', C_Q), (Wk, 'Wk', C_K), (Wv, 'Wv', C_V)):
                            sgt = stg[0] if wn != 'Wk' else stg[1]
                            skey = ('stg', sgt.name if hasattr(sgt, 'name') else id(sgt))
                            P.dma('sp', sgt[:, :].rearrange("p (a b) -> p a b", a=8), w_in_v[:, :, c0 + hcol:c0 + hcol + 128], w=[skey])
                            P.cp('dve', Wt[:], sgt[:, :].rearrange("p (a b) -> p a b", a=8), r=[skey], w=[(wn, kc) for kc in range(8)])
                        for tb in range(4):
                            proj_norm(Wq, 'Wq', TOK + tb * 512, qn_t[:, 0:1], QT[:, tb * 512:(tb + 1) * 512], 'QT')
                        for tb in range(8):
                            proj_norm(Wk, 'Wk', tb * 512, knAB[:, 0:1], KT[:, tb * 512:(tb + 1) * 512], 'KT',
                                      KTB[:, tb * 512:(tb + 1) * 512], knAB[:, 1:2])
                        nbk = 32 // r_
                        for rho in range(r_):
                            for jb in range(nbk):
                                ti = rho * nbk + jb
                                s0 = rho + r_ * 128 * jb
                                for kc in range(8):
                                    lh = hTa[:, kc, s0:s0 + 1]
                                    a_ = list(lh.ap)
                                    lhs = bass.AP(lh.tensor, lh.offset, [list(a_[0]), [r_, 128]])
                                    P.mm(pvb[ti % 2][:, 0:128], lhs, Wv[:, kc, :], start=(kc == 0), stop=(kc == 7),
                                         r=[('hTa', kc), ('Wv', kc)], w=['psB%d' % (ti % 2)])
                                kv = 'psB%d' % (ti % 2)
                                if jb < nbk // 2:
                                    P.ts('dve', VA[:, ti, 0:64], pvb[ti % 2][:, 0:64], hv_t[:, 0:1], None, ALU.mult, r=[kv, 'hv'], w=[('VA', ti)])
                                    P.ts('dve', VB[:, ti, 64:128], pvb[ti % 2][:, 64:128], hv_t[:, 0:1], None, ALU.mult, r=[kv, 'hv'], w=[('VB', ti)])
                                else:
                                    P.cp('dve', VA[:, ti, 0:64], pvb[ti % 2][:, 0:64], r=[kv], w=[('VA', ti)])
                                    P.cp('act', VB[:, ti, 64:128], pvb[ti % 2][:, 64:128], r=[kv], w=[('VB', ti)])
                        for rho in range(r_):
                            for n in range(16 // r_):
                                jbp = 16 // r_ + n - 1
                                tp = rho * nbk + jbp
                                qsl = lambda ph: strided(QT, ph, rho + r_ * 128 * n, r_, 128)
                                ab_ = pcnt[1] % 2
                                pcnt[1] += 1
                                bank = 4 + ab_
                                kS = 'psF%d' % bank
                                PT = PT2[ab_]
                                kP = ('PT', ab_)
                                ob = ab_
                                kO = 'psF%d' % ob
                                for hh in range(2):
                                    ph = slice(0, 128)
                                    h = g * 8 + hp * 2 + hh
                                    for pc in range(2):
                                        col = (hh * 2 + pc) * 128
                                        ksl = strided(KT if hh == 0 else KTB, ph, rho + r_ * 128 * (jbp + pc), r_, 128)
                                        P.mm(psF[bank][:, col:col + 128], ksl, qsl(ph), start=True, stop=False,
                                             r=['KT', 'QT'], w=[kS])
                                        P.mm(psF[bank][:, col:col + 128], identb[:], biasv[:, h * 2 + pc, :], start=False, stop=True,
                                             r=['identb'] + [('biasb', c) for c in range(6)], w=[kS])
                                P.act(PT[:], psF[bank][:, :], AF.Exp, r=[kS], w=[kP])
                                for q4, (Vt, vn, tix) in enumerate(((VA, 'VA', tp), (VA, 'VA', tp + 1), (VB, 'VB', tp), (VB, 'VB', tp + 1))):
                                    P.mm(psF[ob][:, 0:128], Vt[:, tix, :], PT[:, q4 * 128:(q4 + 1) * 128], start=(q4 == 0), stop=(q4 == 3),
                                         r=[(vn, tix), kP], w=[kO])
                                for q4, ei in enumerate((2, 0, 3, 1) if n == 0 else (0, 0, 1, 1)):
                                    P.mm(psF[ob][:, 128:256], Emat[:, ei, :], PT[:, q4 * 128:(q4 + 1) * 128], start=(q4 == 0), stop=(q4 == 3),
                                         r=['E', kP], w=[kO])
                                q0 = rho + r_ * 128 * n
                                ab = acc[:, 0:1, q0:q0 + 1]
                                aa = list(ab.ap)
                                accv = bass.AP(ab.tensor, ab.offset, [list(aa[0]), [TOK, 2], [r_, 128]])
                                pv = psF[ob][:, 0:256].rearrange("p (a q) -> p a q", a=2)
                                if g == 0:
                                    P.cp('dve', accv, pv, r=[kO], w=['acc'])
                                else:
                                    P.tt('dve', accv, accv, pv, ALU.add, r=[kO, 'acc'], w=['acc'])
                    P._add('dve', lambda h, a=acc: h.reciprocal(a[:, 1, :], a[:, 1, :]), ['acc'], ['acc'], False)
                    P.tt('dve', aoT[:, hp, :], acc[:, 0, :], acc[:, 1, :], ALU.mult, r=['acc'], w=[('aoT', hp)])
                if DEBUG is not None and STAGES[-1] == "C":
                    for hp in range(4):
                        P.cp('dve', acc[:, 0, :], aoT[:, hp, :], r=[('aoT', hp)], w=['acc'])
                        P.dma('sp', dbg[hp * 128:(hp + 1) * 128, 0:TOK], acc[:, 0, :], r=['acc'])
                P.barrier()

        if "D" in STAGES:
            with ExitStack() as st:
                stg = [sb("stgD%d" % i, [128, 1024], F32, st) for i in range(2)]
                Wat = sb("Wat", [128, 4, 1024], BF16, st)
                Wga = sb("Wga", [128, 8, 1024], BF16, st)
                Wo = sb("Wo", [128, 8, 1024], BF16, st)
                Wr = sb("Wr", [128, 8, 72], BF16, st)
                wload(Wat, lambda c: w_attn_v[:, c, :], 4, 1024, stg, 'Wat')
                wload(Wga, lambda kc: w_in_v[:, kc, C_GA:C_GA + 1024], 8, 1024, stg, 'Wga')
                wload(Wo, lambda c: w_out_v[:, c, :], 8, 1024, stg, 'Wo')
                wload(Wr, lambda c: w_rt_v[:, c, :], 8, 72, stg, 'Wr')
                hTm = sb("hTmD", [128, 8, TOK], BF16, st)
                for kc in range(8):
                    P.dma('sp', hTm[:, kc, :], hT_s[1, :, kc * TOK:(kc + 1) * TOK], w=[('hTm', kc)])
                nfw_t = sb("nfw_t", [128, D], F32, st)
                brt_t = sb("brt_t", [128, 72], F32, st)
                P.dma('sp', nfw_t[:], nfw_row[0].partition_broadcast(128), w=['nfw'])
                P.dma('sp', brt_t[:], b_rt[0].partition_broadcast(128), w=['brt'])
                xt_ = sb("xtD", [128, D], F32, st)
                mgt = sb("mgtD", [128, D], F32, st)
                sg = sb("sgD", [128, D], F32, st)
                tmp = sb("tmpD", [128, D], F32, st)
                mrg = sb("mrgD", [128, D], BF16, st)
                mT = sb("mTD", [128, 8, 128], BF16, st)
                xmd = sb("xmdD", [128, D], F32, st)
                sq = sb("sqD", [128, D], F32, st)
                hn = sb("hnD", [128, D], BF16, st)
                hnT = sb("hnTD", [128, 8, 128], BF16, st)
                ssd = sb("ssD", [128, 2], F32, st)
                lg = sb("lgD", [128, 72], F32, st)
                rt = sb("rtD", [128, 16, 8], F32, st)
                f3 = sb("f3D", [128, 8, 8], F32, st)
                for i in range(16):
                    ts_ = slice(i * 128, (i + 1) * 128)
                    P.dma('sp', xt_[:], xm[ts_, :], w=['xt'])
                    P.dma('sp', mgt[:], mg_s[ts_, :], w=['mgt'])
                    for cb in range(2):
                        kq = 'psF%d' % cb
                        for hp in range(4):
                            P.mm(psF[cb][:, :], aoT[:, hp, ts_], Wat[:, hp, cb * 512:(cb + 1) * 512], start=(hp == 0), stop=(hp == 3),
                                 r=[('aoT', hp), ('Wat', hp)], w=[kq])
                        kg = 'psF%d' % (2 + cb)
                        for kc in range(8):
                            P.mm(psF[2 + cb][:, :], hTm[:, kc, ts_], Wga[:, kc, cb * 512:(cb + 1) * 512], start=(kc == 0), stop=(kc == 7),
                                 r=[('hTm', kc), ('Wga', kc)], w=[kg])
                        cs2 = slice(cb * 512, (cb + 1) * 512)
                        P.act(sg[:, cs2], psF[2 + cb][:, :], AF.Sigmoid, r=[kg], w=[('sg', cb)])
                        P.tt('dve', tmp[:, cs2], sg[:, cs2], psF[cb][:, :], ALU.mult, r=[('sg', cb), kq], w=[('tmp', cb)])
                        P.tt('pool', mrg[:, cs2], tmp[:, cs2], mgt[:, cs2], ALU.add, r=[('tmp', cb), 'mgt'], w=[('mrg', cb)])
                    for c in range(8):
                        P.tr(psB[0][:, c * 128:(c + 1) * 128], mrg[:, c * 128:(c + 1) * 128], identb[:],
                             r=[('mrg', c // 4), 'identb'], w=['psB0'])
                    P.cp('dve', mT[:], psB[0][:, :].rearrange("p (a b) -> p a b", a=8), r=['psB0'], w=['mT'])
                    for cb in range(2):
                        kq = 'psF%d' % (4 + cb)
                        cs2 = slice(cb * 512, (cb + 1) * 512)
                        for c in range(8):
                            P.mm(psF[4 + cb][:, :], mT[:, c, :], Wo[:, c, cs2], start=(c == 0), stop=(c == 7),
                                 r=['mT', ('Wo', c)], w=[kq])
                        P.tt('dve', xmd[:, cs2], xt_[:, cs2], psF[4 + cb][:, :], ALU.add, r=['xt', kq], w=[('xmd', cb)])
                    xk = [('xmd', 0), ('xmd', 1)]
                    P.dma('sp', xmid_s[ts_, :], xmd[:], r=xk)
                    if DEBUG is not None and STAGES[-1] == "D":
                        P.dma('sp', dbg[ts_, 0:1024], xmd[:], r=xk)
                    P.tt('pool', sq[:], xmd[:], xmd[:], ALU.mult, r=xk, w=['sq'])
                    P.red('dve', ssd[:, 0:1], sq[:], ALU.add, r=['sq'], w=['ssd0'])
                    rstd_ops(ssd[:, 1:2], ssd[:, 0:1], 1.0 / D, ['ssd0'], ['ssd1'])
                    P.stt('dve', hn[:], xmd[:], ssd[:, 1:2], nfw_t[:], ALU.mult, ALU.mult, r=xk + ['ssd1', 'nfw'], w=['hn'])
                    P.dma('sp', hn_s[ts_, :], hn[:], r=['hn'])
                    for c in range(8):
                        P.tr(psB[1][:, c * 128:(c + 1) * 128], hn[:, c * 128:(c + 1) * 128], identb[:], r=['hn', 'identb'], w=['psB1'])
                    P.cp('act', hnT[:], psB[1][:, :].rearrange("p (a b) -> p a b", a=8), r=['psB1'], w=['hnT'])
                    for kc in range(8):
                        P.mm(psF[0][:, 0:72], hnT[:, kc, :], Wr[:, kc, :], start=(kc == 0), stop=(kc == 7),
                             r=['hnT', ('Wr', kc)], w=['psF0'])
                    P.tt('dve', lg[:], psF[0][:, 0:72], brt_t[:], ALU.add, r=['psF0', 'brt'], w=['lg'])
                    m, negm, ohg, ex, se, gp, sel, m1, oh1, sel2, m2, oh2, dd, s1 = [rt[:, k, :] for k in range(14)]
                    K_ = ['lg', 'rt']
                    P.red('dve', m[:, 0:1], lg[:, 0:8], ALU.max, r=['lg'], w=['rt'])
                    P.ts('dve', negm[:, 0:1], m[:, 0:1], -1.0, None, ALU.mult, r=['rt'], w=['rt'])
                    P.ts('dve', ohg, lg[:, 0:8], m[:, 0:1], None, ALU.is_equal, r=K_, w=['rt'])
                    P.act(ex, lg[:, 0:8], AF.Exp, bias=negm[:, 0:1], r=K_, w=['rt'])
                    P.red('dve', se[:, 0:1], ex, ALU.add, r=['rt'], w=['rt'])
                    P._add('dve', lambda h, a=gp[:, 0:1], b_=se[:, 0:1]: h.reciprocal(a, b_), ['rt'], ['rt'], False)
                    P.tt('dve', f3[:], lg[:, 8:72].rearrange("p (g e) -> p g e", g=8), bc_free(ohg, 8), ALU.mult, r=K_, w=['f3'])
                    P.red('dve', sel, f3[:].rearrange("p g e -> p e g"), ALU.add, r=['f3'], w=['rt'])
                    P.red('dve', m1[:, 0:1], sel, ALU.max, r=['rt'], w=['rt'])
                    P.ts('dve', oh1, sel, m1[:, 0:1], None, ALU.is_equal, r=['rt'], w=['rt'])
                    P.stt('dve', sel2, oh1, -1.0e9, sel, ALU.mult, ALU.add, r=['rt'], w=['rt'])
                    P.red('dve', m2[:, 0:1], sel2, ALU.max, r=['rt'], w=['rt'])
                    P.ts('dve', oh2, sel2, m2[:, 0:1], None, ALU.is_equal, r=['rt'], w=['rt'])
                    P.tt('dve', dd[:, 0:1], m1[:, 0:1], m2[:, 0:1], ALU.subtract, r=['rt'], w=['rt'])
                    P.act(s1[:, 0:1], dd[:, 0:1], AF.Sigmoid, r=['rt'], w=['rt'])
                    P.tt('dve', gts[:, i, 0:1], gp[:, 0:1], s1[:, 0:1], ALU.mult, r=['rt'], w=['gts'])
                    P.tt('dve', gts[:, i, 1:2], gp[:, 0:1], gts[:, i, 0:1], ALU.subtract, r=['rt', 'gts'], w=['gts'])
                    P.tt('dve', ohA[:, i, :].rearrange("p (g e) -> p g e", g=8), bc_free(ohg, 8), bc_mid(oh1, 8), ALU.mult,
                         r=['rt'], w=['ohA'])
                    P.tt('dve', ohB[:, i, :].rearrange("p (g e) -> p g e", g=8), bc_free(ohg, 8), bc_mid(oh2, 8), ALU.mult,
                         r=['rt'], w=['ohB'])
                P.barrier()

        if "E" in STAGES:
            with ExitStack() as st:
                zi = sb("ziE", [128, 64], I32, st)
                P.memset('dve', zi[:], 0, w=['zi'])
                P.dma('sp', idx_s[0:NE * CAP, :].rearrange("(p c) o -> p (c o)", p=128), zi[:], r=['zi'], w=['idx_s'])
                zf = sb("zfE", [128, D], F32, st)
                P.memset('pool', zf[:], 0.0, w=['zf'])
                P.dma('sp', yrow_s[NE * CAP:NE * CAP + 128, :], zf[:], r=['zf'])
                Racc = sb("Racc", [128, 64], F32, st)
                Ms = sb("MsE", [128, 64], F32, st)
                slot = sb("slotE", [128, 64], F32, st)
                ov = sb("ovE", [128, 64], F32, st)
                t2 = sb("t2E", [128, 64], F32, st)
                df = sb("dfE", [128, 2], F32, st)
                P.memset('dve', Racc[:], 0.0, w=['Racc'])
                Msb = sb("MsbE", [128, 2, 64], BF16, st)
                SUb = sb("SUbE", [128, 128], BF16, st)
                P.cp('dve', SUb[:], SUmat, r=['cst2'], w=['SUb'])
                for i in range(16):
                    P.tt('dve', Ms[:], ohA[:, i, :], ohB[:, i, :], ALU.add, r=['ohA', 'ohB'], w=['Ms'])
                    P.cp('dve', Msb[:, 0, :], Ms[:], r=['Ms'], w=['Msb'])
                    P.cp('dve', Msb[:, 1, :], Racc[:], r=['Racc'], w=['Msb'])
                    P.mm(psF[0][:, 0:64], SUb[:], Msb[:, 0, :], start=True, stop=False, r=['SUb', 'Msb'], w=['psF0'])
                    P.mm(psF[0][:, 0:64], onesb, Msb[:, 1, :], start=False, stop=True, r=['cst', 'Msb'], w=['psF0'])
                    P.ts('dve', ov[:], psF[0][:, 0:64], float(CAP), 1.0e6, ALU.is_ge, ALU.mult, r=['psF0'], w=['ov'])
                    P.tt('dve', slot[:], psF[0][:, 0:64], iotae, ALU.add, r=['psF0', 'cst2'], w=['slot'])
                    P.tt('dve', slot[:], slot[:], ov[:], ALU.add, r=['slot', 'ov'], w=['slot'])
                    for k, (oh, dst) in enumerate(((ohA, dstA), (ohB, dstB))):
                        P.tt('dve', t2[:], oh[:, i, :], slot[:], ALU.mult, r=['ohA', 'ohB', 'slot'], w=['t2'])
                        P.red('dve', df[:, k:k + 1], t2[:], ALU.add, r=['t2'], w=['df'])
                        P.ts('dve', df[:, k:k + 1], df[:, k:k + 1], float(NE * CAP), None, ALU.min, r=['df'], w=['df'])
                        P.cp('dve', dst[:, i:i + 1], df[:, k:k + 1], r=['df'], w=['dst%d' % k])
                        P.idma(idx_s[:, :], bass.IndirectOffsetOnAxis(ap=dst[:, i:i + 1], axis=0), tokid[:, i:i + 1], None,
                               NE * CAP - 1, r=['dst%d' % k, 'tokid'], w=['idx_s'])
                    P.tt('pool', Racc[:], Racc[:], Ms[:], ALU.add, r=['Racc', 'Ms'], w=['Racc'])
                P.barrier()

        if "F" in STAGES:
            with ExitStack() as st:
                sG = [sb("sG%d" % i, [128, 8, 512], F32, st) for i in range(2)]
                sU = [sb("sU%d" % i, [128, 8, 512], F32, st) for i in range(2)]
                sD = [sb("sD%d" % i, [128, 4, 1024], F32, st) for i in range(2)]
                bG = [sb("bG%d" % i, [128, 8, 512], BF16, st) for i in range(2)]
                bU = [sb("bU%d" % i, [128, 8, 512], BF16, st) for i in range(2)]
                bD = [sb("bD%d" % i, [128, 4, 1024], BF16, st) for i in range(2)]
                idt = [sb("idt%d" % i, [128, 1], I32, st) for i in range(2)]
                xe = [sb("xe%d" % i, [128, D], BF16, st) for i in range(2)]
                xeT = sb("xeT", [128, 8, 128], BF16, st)
                gsl = sb("gslF", [128, 512], F32, st)
                hid = sb("hidF", [128, 512], BF16, st)
                hidT = sb("hidT", [128, 4, 128], BF16, st)
                yr = sb("yrF", [128, D], F32, st)
                def f_dma(e):
                    p2 = e % 2
                    P.dma('sp', sG[p2][:], w_g[e].rearrange("(kc p) n -> p kc n", p=128), w=[('sG', p2)])
                    P.dma('sp', sU[p2][:], w_u[e].rearrange("(kc p) n -> p kc n", p=128), w=[('sU', p2)])
                    P.dma('sp', sD[p2][:], w_d[e].rearrange("(c p) n -> p c n", p=128), w=[('sD', p2)])
                    P.dma('sp', idt[p2][:], idx_s[e * CAP:(e + 1) * CAP, :], w=[('idt', p2)])
                    P.idma(xe[p2][:, :], None, hn_s[:, :], bass.IndirectOffsetOnAxis(ap=idt[p2][:, :], axis=0), None,
                           r=[('idt', p2)], w=[('xe', p2)])

                def f_cast(e):
                    p2 = e % 2
                    P.cp('dve', bG[p2][:], sG[p2][:], r=[('sG', p2)], w=[('bG', p2)])
                    P.cp('pool', bU[p2][:], sU[p2][:], r=[('sU', p2)], w=[('bU', p2)])
                    P.cp('act', bD[p2][:], sD[p2][:], r=[('sD', p2)], w=[('bD', p2)])

                f_dma(0)
                f_cast(0)
                f_dma(1)
                for e in range(NE):
                    p2 = e % 2
                    for c in range(8):
                        P.tr(psB[0][:, c * 128:(c + 1) * 128], xe[p2][:, c * 128:(c + 1) * 128], identb[:],
                             r=[('xe', p2), 'identb'], w=['psB0'])
                    P.cp('dve', xeT[:], psB[0][:, :].rearrange("p (a b) -> p a b", a=8), r=['psB0'], w=['xeT'])
                    for kc in range(8):
                        P.mm(psF[0][:, :], xeT[:, kc, :], bG[p2][:, kc, :], start=(kc == 0), stop=(kc == 7), r=['xeT', ('bG', p2)], w=['psF0'])
                    for kc in range(8):
                        P.mm(psF[1][:, :], xeT[:, kc, :], bU[p2][:, kc, :], start=(kc == 0), stop=(kc == 7), r=['xeT', ('bU', p2)], w=['psF1'])
                    P.act(gsl[:], psF[0][:, :], AF.Silu, r=['psF0'], w=['gsl'])
                    P.tt('dve', hid[:], gsl[:], psF[1][:, :], ALU.mult, r=['gsl', 'psF1'], w=['hid'])
                    for c in range(4):
                        P.tr(psB[1][:, c * 128:(c + 1) * 128], hid[:, c * 128:(c + 1) * 128], identb[:], r=['hid', 'identb'], w=['psB1'])
                    P.cp('act', hidT[:], psB[1][:, 0:512].rearrange("p (a b) -> p a b", a=4), r=['psB1'], w=['hidT'])
                    for cb in range(2):
                        kq = 'psF%d' % (2 + cb)
                        for c in range(4):
                            P.mm(psF[2 + cb][:, :], hidT[:, c, :], bD[p2][:, c, cb * 512:(cb + 1) * 512], start=(c == 0), stop=(c == 3),
                                 r=['hidT', ('bD', p2)], w=[kq])
                        P.cp('dve' if cb == 0 else 'act', yr[:, cb * 512:(cb + 1) * 512], psF[2 + cb][:, :], r=[kq], w=[('yr', cb)])
                    P.dma('act', yrow_s[e * CAP:(e + 1) * CAP, :], yr[:], r=[('yr', 0), ('yr', 1)])
                    if e + 1 < NE:
                        f_cast(e + 1)
                    if e + 2 < NE:
                        f_dma(e + 2)
                P.barrier()

        if "G" in STAGES:
            with ExitStack() as st:
                xmt = [sb("xmtG%d" % i, [128, D], F32, st) for i in range(2)]
                ya = [sb("yaG%d" % i, [128, D], F32, st) for i in range(2)]
                yb = [sb("ybG%d" % i, [128, D], F32, st) for i in range(2)]
                o1 = [sb("o1G%d" % i, [128, D], F32, st) for i in range(2)]
                for i in range(16):
                    p2 = i % 2
                    ts_ = slice(i * 128, (i + 1) * 128)
                    P.dma('sp', xmt[p2][:], xmid_s[ts_, :], w=[('xmt', p2)])
                    P.memset('pool', ya[p2][:], 0.0, w=[('ya', p2)])
                    P.memset('pool', yb[p2][:], 0.0, w=[('yb', p2)])
                    P.idma(ya[p2][:, :], None, yrow_s[:, :], bass.IndirectOffsetOnAxis(ap=dstA[:, i:i + 1], axis=0), None,
                           r=['dst0'], w=[('ya', p2)])
                    P.idma(yb[p2][:, :], None, yrow_s[:, :], bass.IndirectOffsetOnAxis(ap=dstB[:, i:i + 1], axis=0), None,
                           r=['dst1'], w=[('yb', p2)])
                    P.stt('dve', o1[p2][:], ya[p2][:], gts[:, i, 0:1], xmt[p2][:], ALU.mult, ALU.add,
                          r=[('ya', p2), ('xmt', p2), 'gts'], w=[('o1', p2)])
                    P.stt('dve', o1[p2][:], yb[p2][:], gts[:, i, 1:2], o1[p2][:], ALU.mult, ALU.add,
                          r=[('yb', p2), ('o1', p2), 'gts'], w=[('o1', p2)])
                    P.dma('sp', out[ts_, :], o1[p2][:], r=[('o1', p2)])

        P.final_wait('sp')
        with ExitStack() as es:
            sems = {}
            for s in sorted(P.semnames):
                sems[s] = es.enter_context(nc.semaphore("s_" + s))
            block = es.enter_context(nc.Block())
            P.emit(block, sems)
    return nc


def _host_consts():
    i = np.arange(128)
    ident = np.eye(128, dtype=np.float32)
    U = (i[:, None] <= i[None, :]).astype(np.float32)
    SL = (i[:, None] > i[None, :]).astype(np.float32)
    ones = np.ones((128, 128), np.float32)
    return np.concatenate([ident, U, SL, ones], axis=1)


def _host_consts2():
    i = np.arange(128)
    su = (i[:, None] < i[None, :]).astype(np.float32)
    blk = ((i[:, None] // 64) == (i[None, :] // 64)).astype(np.float32)
    iot = np.tile((np.arange(64, dtype=np.float32) * CAP)[None, :], (128, 1))
    return np.ascontiguousarray(np.concatenate([su, blk, iot], axis=1))


def _t5_bucket(dist):
    nb, md = 32, 2048
    me = nb // 2
    large = me + (np.log(np.maximum(dist, me) / me) / np.log(md / me) * (nb - me)).astype(np.int32)
    return np.where(dist < me, dist, np.minimum(large, nb - 1)).astype(np.int32)


def prep_inputs(inp):
    f = np.float32
    x = np.asarray(inp['x'], f)
    cst = _host_consts()

    def chunked(v, n):
        return np.ascontiguousarray(np.asarray(v, f).reshape(n, 128).T)
    conv_w = np.asarray(inp['conv_w'], f)[0]
    cw = np.ascontiguousarray(conv_w.reshape(4, 20, 128).transpose(2, 0, 1))
    cb = chunked(np.asarray(inp['conv_b'])[0], 20)
    dsk = np.ascontiguousarray(np.repeat(np.asarray(inp['d_skip'], f)[0], 64).reshape(16, 128).T)
    qnw = np.tile(np.asarray(inp['q_norm_w'], f)[0], 2).reshape(128, 1)
    knw = np.tile(np.asarray(inp['k_norm_w'], f)[0], 2).reshape(128, 1)
    rb = np.asarray(inp['rel_bias'], f)
    kq = np.arange(128)
    biasT = np.full((128, 24, 2, 128), -30000.0, f)
    for g, r in enumerate((1, 4, 16)):
        for hh in range(8):
            h = g * 8 + hh
            offp = kq[None, :] + 128 - kq[:, None]
            offc = kq[None, :] - kq[:, None]
            tp = rb[_t5_bucket(np.clip(offp, 0, None) * r), h]
            tcur = rb[_t5_bucket(np.clip(offc, 0, None) * r), h]
            biasT[:, h, 0, :] = np.where((offp >= 0) & (offp <= 128), tp, -30000.0)
            biasT[:, h, 1, :] = np.where((offc >= 0) & (offc <= 128), tcur, -30000.0)
    w_rt = np.concatenate([np.asarray(inp['w_coarse'], f)[0], np.asarray(inp['w_fine'], f)[0]], axis=1)
    b_rt = np.concatenate([np.asarray(inp['b_coarse'], f)[0], np.asarray(inp['b_fine'], f)[0]])[None, :]
    shared = {
        'cst': cst, 'w_in': np.asarray(inp['w_in'], f)[0], 'nmw': chunked(np.asarray(inp['norm_mix_w'])[0], 8),
        'conv_w': cw, 'conv_b': cb, 'dtb': np.asarray(inp['dt_bias'], f), 'alog': np.asarray(inp['a_log'], f),
        'dsk': dsk, 'snw': np.asarray(inp['ssm_norm_w'], f), 'w_ssm': np.asarray(inp['w_ssm_proj'], f)[0],
        'qnw': qnw, 'knw': knw, 'biasT': np.ascontiguousarray(biasT.reshape(128, -1)),
        'w_attn': np.asarray(inp['w_attn_proj'], f)[0], 'w_out': np.asarray(inp['w_out'], f)[0],
        'nfw': chunked(np.asarray(inp['norm_ffn_w'])[0], 8), 'w_rt': np.ascontiguousarray(w_rt), 'b_rt': b_rt,
        'nfw_row': np.asarray(inp['norm_ffn_w'], f), 'cst2': _host_consts2(),
        'tokid_d': np.ascontiguousarray(np.arange(16, dtype=np.int32)[None, :] * 128 + np.arange(128, dtype=np.int32)[:, None]),
        'w_g': np.asarray(inp['w_gate_exp'], f)[0][:(NE if "F" in STAGES else 1)],
        'w_u': np.asarray(inp['w_up_exp'], f)[0][:(NE if "F" in STAGES else 1)],
        'w_d': np.asarray(inp['w_down_exp'], f)[0][:(NE if "F" in STAGES else 1)],
    }
    maps = []
    for c in range(8):
        b, q = c // 4, c % 4
        m = dict(shared)
        m['xm'] = np.ascontiguousarray(x[b, q * TOK:(q + 1) * TOK])
        xpv = np.zeros((PRE, D), f)
        if q > 0:
            xpv[PRE - q * TOK:] = x[b, 0:q * TOK]
        m['xp'] = xpv
        pmv = np.ones((128, NBLK), f)
        for j in range(12):
            pmv[:, j] = 1.0 if j * 512 >= PRE - q * TOK else 0.0
        m['pm'] = pmv
        m['hv'] = np.full((128, 1), 1.0 if q > 0 else 0.0, f)
        maps.append(m)
    return maps


def kernel(**inputs):
    maps = prep_inputs(inputs)
    nc = build_program()
    res = run_bass_kernel_spmd(nc, maps, core_ids=list(range(8)))
    outs = [np.asarray(r['out']) for r in res.results]
    full = np.stack([np.concatenate(outs[0:4], 0), np.concatenate(outs[4:8], 0)], 0)
    return full.astype(np.float32)
```
